# Optimizing a Trainium2 kernel written in Bass

```python
import jax, jax.numpy as jnp
from jax import lax
import numpy as np

D_MODEL = 1024
BATCH = 8
SEQ = 8192
DEPTH = 4

CHUNK = 64
CONV_CH = 512
CONV_K = 31
RET_HEADS = 4
RET_DK = 64
RET_DV = 128
RET_QK_W = RET_HEADS * RET_DK
RET_V_W = RET_HEADS * RET_DV
MIX_W = CONV_CH + RET_V_W
IN_W = 2 * CONV_CH + 2 * RET_QK_W + 2 * RET_V_W
ROPE_BASE = 10000.0
D_FF = 3584
N_EXPERTS = 8
TOP_K = 2
MOE_BLOCK = 256
N_DENSE = (DEPTH + 1) // 2
N_MOE = DEPTH // 2
ALPHA = (2.0 * DEPTH) ** 0.25
BETA = (8.0 * DEPTH) ** -0.25
LN_EPS = 1e-5

kernel_name = 'hybrid_conv_retention_moe_block'


def layer_norm(x, g, b):
    xf = x.astype(jnp.float32)
    mu = jnp.mean(xf, axis=-1, keepdims=True)
    var = jnp.mean(jnp.square(xf - mu), axis=-1, keepdims=True)
    return ((xf - mu) * lax.rsqrt(var + LN_EPS) * g + b).astype(x.dtype)


def rotary(x, pos):
    half = x.shape[-1] // 2
    inv_freq = ROPE_BASE ** (-jnp.arange(half, dtype=jnp.float32) / half)
    ang = pos.astype(jnp.float32)[:, None] * inv_freq[None, :]
    cos = jnp.cos(ang)[None, :, None, :]
    sin = jnp.sin(ang)[None, :, None, :]
    x1, x2 = x[..., :half], x[..., half:]
    return jnp.concatenate([x1 * cos - x2 * sin, x1 * sin + x2 * cos], axis=-1)


def retention_chunkwise(q, k, v):
    b, s, h, dk = q.shape
    dv = v.shape[-1]
    nc = s // CHUNK
    log_gamma = jnp.log1p(-(2.0 ** (-5.0 - jnp.arange(h, dtype=jnp.float32))))
    qc = q.reshape(b, nc, CHUNK, h, dk)
    kc = k.reshape(b, nc, CHUNK, h, dk)
    vc = v.reshape(b, nc, CHUNK, h, dv)
    idx = jnp.arange(CHUNK, dtype=jnp.float32)
    intra_decay = jnp.exp(log_gamma[:, None, None] * jnp.abs(idx[:, None] - idx[None, :]))
    scores = jnp.einsum('bnihd,bnjhd->bnhij', qc, kc) * intra_decay
    intra = jnp.einsum('bnhij,bnjhe->bnihe', scores, vc)
    k_decay = jnp.exp(log_gamma[None, :] * (CHUNK - 1.0 - idx)[:, None])
    kv = jnp.einsum('bnjhd,bnjhe->nbhde', kc * k_decay[:, :, None], vc)
    chunk_decay = jnp.exp(log_gamma * CHUNK)[None, :, None, None]

    def step(state, kv_n):
        return state * chunk_decay + kv_n, state

    _, prev = lax.scan(step, jnp.zeros((b, h, dk, dv), jnp.float32), kv)
    q_decay = jnp.exp(log_gamma[None, :] * (idx + 1.0)[:, None])
    cross = jnp.einsum('bnihd,nbhde->bnihe', qc * q_decay[:, :, None], prev)
    return (intra + cross).reshape(b, s, h, dv)


def hybrid_mixer(x, w_in, w_out, conv_w, conv_b, conv_ln_g, conv_ln_b, ret_gn_g, pos):
    b, s, _ = x.shape
    proj = jnp.einsum('bsd,df->bsf', x, w_in)
    cuts = (CONV_CH, 2 * CONV_CH, 2 * CONV_CH + RET_QK_W,
            2 * CONV_CH + 2 * RET_QK_W, 2 * CONV_CH + 2 * RET_QK_W + RET_V_W)
    glu_a, glu_b, q, k, v, g = jnp.split(proj, cuts, axis=-1)
    u = glu_a * jax.nn.sigmoid(glu_b)
    u = lax.conv_general_dilated(
        u, conv_w[:, None, :].astype(u.dtype), window_strides=(1,),
        padding=[(CONV_K - 1, 0)], dimension_numbers=('NWC', 'WIO', 'NWC'),
        feature_group_count=CONV_CH) + conv_b
    u = jax.nn.silu(layer_norm(u, conv_ln_g, conv_ln_b))
    qh = rotary(q.astype(jnp.float32).reshape(b, s, RET_HEADS, RET_DK), pos)
    kh = rotary(k.astype(jnp.float32).reshape(b, s, RET_HEADS, RET_DK), pos) * (RET_DK ** -0.5)
    vh = v.astype(jnp.float32).reshape(b, s, RET_HEADS, RET_DV)
    r = retention_chunkwise(qh, kh, vh)
    mu = jnp.mean(r, axis=-1, keepdims=True)
    var = jnp.mean(jnp.square(r - mu), axis=-1, keepdims=True)
    r = ((r - mu) * lax.rsqrt(var + LN_EPS)).reshape(b, s, RET_V_W) * ret_gn_g
    r = (jax.nn.silu(g.astype(jnp.float32)) * r).astype(x.dtype)
    mixed = jnp.concatenate([u.astype(x.dtype), r], axis=-1)
    return jnp.einsum('bsm,md->bsd', mixed, w_out)


def swiglu(h, w1, w3, w2):
    a = jnp.einsum('bsd,df->bsf', h, w1)
    c = jnp.einsum('bsd,df->bsf', h, w3)
    return jnp.einsum('bsf,fd->bsd', jax.nn.silu(a) * c, w2)


def moe_swiglu(h, router, w1, w3, w2):
    n_tok, d = h.shape
    n_assign = n_tok * TOP_K
    n_slots = -(-n_assign // MOE_BLOCK) * MOE_BLOCK + N_EXPERTS * MOE_BLOCK
    n_blocks = n_slots // MOE_BLOCK
    logits = jnp.einsum('nd,de->ne', h, router).astype(jnp.float32)
    top_logit, top_e = lax.top_k(logits, TOP_K)
    gate = jax.nn.softmax(top_logit, axis=-1)
    flat_e = top_e.reshape(-1).astype(jnp.int32)
    flat_tok = jnp.arange(n_assign, dtype=jnp.int32) // TOP_K
    flat_gate = gate.reshape(-1)
    order = jnp.argsort(flat_e)
    sorted_e = flat_e[order]
    counts = jnp.bincount(flat_e, length=N_EXPERTS).astype(jnp.int32)
    padded = (counts + MOE_BLOCK - 1) // MOE_BLOCK * MOE_BLOCK
    start = jnp.cumsum(counts) - counts
    pad_end = jnp.cumsum(padded)
    pad_start = pad_end - padded
    slot = pad_start[sorted_e] + jnp.arange(n_assign, dtype=jnp.int32) - start[sorted_e]
    slot_tok = jnp.zeros((n_slots,), jnp.int32).at[slot].set(flat_tok[order])
    slot_gate = jnp.zeros((n_slots,), jnp.float32).at[slot].set(flat_gate[order])
    block_start = jnp.arange(n_blocks, dtype=jnp.int32) * MOE_BLOCK
    block_e = jnp.minimum(jnp.searchsorted(pad_end, block_start, side='right'), N_EXPERTS - 1)
    xb = h[slot_tok].reshape(n_blocks, MOE_BLOCK, d)

    def expert_block(args):
        xe, e = args
        a = xe @ w1[e]
        c = xe @ w3[e]
        return (jax.nn.silu(a) * c) @ w2[e]

    yb = lax.map(expert_block, (xb, block_e)).reshape(n_slots, d)
    yb = yb * slot_gate[:, None].astype(yb.dtype)
    return jax.ops.segment_sum(yb, slot_tok, num_segments=n_tok).astype(h.dtype)


def setup_inputs(seed: int = 0) -> dict:
    key = jax.random.key(seed)
    ks = jax.random.split(key, 20)

    def nrm(k, shape, scale):
        return jax.random.normal(k, shape, jnp.float32) * scale

    return {
        'x': nrm(ks[0], (BATCH, SEQ, D_MODEL), 1.0),
        'w_in': nrm(ks[1], (DEPTH, D_MODEL, IN_W), D_MODEL ** -0.5),
        'w_out': nrm(ks[2], (DEPTH, MIX_W, D_MODEL), BETA * MIX_W ** -0.5),
        'conv_w': nrm(ks[3], (DEPTH, CONV_K, CONV_CH), CONV_K ** -0.5),
        'conv_b': nrm(ks[4], (DEPTH, CONV_CH), 0.02),
        'conv_ln_g': 1.0 + nrm(ks[5], (DEPTH, CONV_CH), 0.02),
        'conv_ln_b': nrm(ks[6], (DEPTH, CONV_CH), 0.02),
        'ret_gn_g': 1.0 + nrm(ks[7], (DEPTH, RET_V_W), 0.02),
        'ln1_g': 1.0 + nrm(ks[8], (DEPTH, D_MODEL), 0.02),
        'ln1_b': nrm(ks[9], (DEPTH, D_MODEL), 0.02),
        'ln2_g': 1.0 + nrm(ks[10], (DEPTH, D_MODEL), 0.02),
        'ln2_b': nrm(ks[11], (DEPTH, D_MODEL), 0.02),
        'dense_w1': nrm(ks[12], (N_DENSE, D_MODEL, D_FF), D_MODEL ** -0.5),
        'dense_w3': nrm(ks[13], (N_DENSE, D_MODEL, D_FF), D_MODEL ** -0.5),
        'dense_w2': nrm(ks[14], (N_DENSE, D_FF, D_MODEL), BETA * D_FF ** -0.5),
        'moe_router': nrm(ks[15], (N_MOE, D_MODEL, N_EXPERTS), D_MODEL ** -0.5),
        'moe_w1': nrm(ks[16], (N_MOE, N_EXPERTS, D_MODEL, D_FF), D_MODEL ** -0.5),
        'moe_w3': nrm(ks[17], (N_MOE, N_EXPERTS, D_MODEL, D_FF), D_MODEL ** -0.5),
        'moe_w2': nrm(ks[18], (N_MOE, N_EXPERTS, D_FF, D_MODEL), BETA * D_FF ** -0.5),
    }


def reference(x, w_in, w_out, conv_w, conv_b, conv_ln_g, conv_ln_b, ret_gn_g,
              ln1_g, ln1_b, ln2_g, ln2_b, dense_w1, dense_w3, dense_w2,
              moe_router, moe_w1, moe_w3, moe_w2):
    b, s, d = x.shape
    pos = jnp.arange(s, dtype=jnp.int32)
    for l in range(DEPTH):
        mix = hybrid_mixer(x, w_in[l], w_out[l], conv_w[l], conv_b[l],
                           conv_ln_g[l], conv_ln_b[l], ret_gn_g[l], pos)
        x = layer_norm(ALPHA * x + mix, ln1_g[l], ln1_b[l])
        i = l // 2
        if l % 2 == 0:
            f = swiglu(x, dense_w1[i], dense_w3[i], dense_w2[i])
        else:
            f = moe_swiglu(x.reshape(b * s, d), moe_router[i], moe_w1[i],
                           moe_w3[i], moe_w2[i]).reshape(b, s, d)
        x = layer_norm(ALPHA * x + f, ln2_g[l], ln2_b[l])
    return x
```

```python
import os
import numpy as np
import concourse.bass as bass
import concourse.mybir as mybir
from concourse.bass_utils import run_bass_kernel_spmd

F32 = mybir.dt.float32
BF16 = mybir.dt.bfloat16
AF = mybir.ActivationFunctionType
ALU = mybir.AluOpType
AX = mybir.AxisListType

D_MODEL = 1024
DEPTH = 4
CHUNK = 64
CONV_CH = 512
CONV_K = 31
RET_HEADS = 4
RET_DK = 64
RET_DV = 128
D_FF = 3584
N_EXPERTS = 8
ALPHA = (2.0 * DEPTH) ** 0.25
LN_EPS = 1e-5
NSLAB = D_FF // 512


class Buf:
    __slots__ = ("name", "w", "r", "excl")

    def __init__(self, name, excl=False):
        self.name = name
        self.w = None
        self.r = {}
        self.excl = excl


class Sched:
    ENGS = ("pe", "dve", "act", "pool", "sp")

    def __init__(self, nc, n_dma_sems=8):
        self.nc = nc
        self.ops = {e: [] for e in self.ENGS}
        self.cnt = {e: 0 for e in self.ENGS}
        self.seen = {e: {} for e in self.ENGS}
        self.sems = {}
        self.n_dma_sems = n_dma_sems
        self.dma_issue = {}
        self.pending = {e: False for e in self.ENGS}
        self.ninstr = 0
        self.log = {e: [] for e in self.ENGS}

    def sem(self, key):
        if key not in self.sems:
            nm = "s_" + "_".join(str(k) for k in (key if isinstance(key, tuple) else (key,)))
            self.sems[key] = self.nc.alloc_semaphore(nm)
        return self.sems[key]

    def _wait(self, eng, key, val):
        if val <= 0 or self.seen[eng].get(key, 0) >= val:
            return
        self.seen[eng][key] = val
        sem = self.sem(key)
        self.ops[eng].append(lambda e, sem=sem, val=val: e.wait_ge(sem, val))
        self.log[eng].append(("w", key, val))
        self.ninstr += 1

    def _deps(self, eng, reads, writes, skip_self):
        deps = {}
        for b in reads:
            if b.w is not None and deps.get(b.w[0], 0) < b.w[1]:
                deps[b.w[0]] = b.w[1]
            if b.excl:
                for k, v in b.r.items():
                    if k != eng and deps.get(k, 0) < v:
                        deps[k] = v
        for b in writes:
            if b.w is not None and deps.get(b.w[0], 0) < b.w[1]:
                deps[b.w[0]] = b.w[1]
            for k, v in b.r.items():
                if deps.get(k, 0) < v:
                    deps[k] = v
        for k, v in deps.items():
            if k == eng and skip_self:
                continue
            self._wait(eng, k, v)

    def _record(self, ev, reads, writes):
        for b in reads:
            if b.r.get(ev[0], 0) < ev[1]:
                b.r[ev[0]] = ev[1]
        for b in writes:
            b.w = ev
            b.r = {}

    def op(self, eng, fn, reads=(), writes=(), inc=True):
        self._deps(eng, reads, writes, skip_self=(eng == "pe"))
        if inc:
            self.cnt[eng] += 1
            ev = (eng, self.cnt[eng])
            sem = self.sem(eng)
            self.ops[eng].append(lambda e, fn=fn, sem=sem: fn(e).then_inc(sem, 1))
            self.log[eng].append(("i", eng, 1))
            self.pending[eng] = False
        else:
            ev = (eng, self.cnt[eng] + 1)
            self.ops[eng].append(lambda e, fn=fn: fn(e))
            self.pending[eng] = True
        self.ninstr += 1
        self._record(ev, reads, writes)
        return ev

    def dma(self, queue, out, in_, reads=(), writes=()):
        i = self.dma_issue.get(queue, 0)
        self.dma_issue[queue] = i + 1
        slot, use = i % self.n_dma_sems, i // self.n_dma_sems
        key = ("dma", queue, slot)
        self._wait(queue, key, 16 * use)
        self._deps(queue, reads, writes, skip_self=False)
        sem = self.sem(key)
        ev = (key, 16 * (use + 1))
        self.ops[queue].append(lambda e, out=out, in_=in_, sem=sem: e.dma_start(out=out, in_=in_).then_inc(sem, 16))
        self.log[queue].append(("i", key, 16))
        self.ninstr += 1
        self._record(ev, reads, writes)
        return ev

    def wait_all(self, eng, bufs):
        self._deps(eng, (), bufs, skip_self=False)

    def check_deadlock(self):
        val = {}
        pos = {e: 0 for e in self.ENGS}
        progress = True
        while progress:
            progress = False
            for e in self.ENGS:
                lg = self.log[e]
                while pos[e] < len(lg):
                    kind, key, v = lg[pos[e]]
                    if kind == "w":
                        if val.get(key, 0) < v:
                            break
                    else:
                        val[key] = val.get(key, 0) + v
                    pos[e] += 1
                    progress = True
        bad = {e: (pos[e], len(self.log[e]), self.log[e][pos[e]]) for e in self.ENGS if pos[e] < len(self.log[e])}
        return bad

    def emit(self):
        for e in ("pe", "dve", "act", "pool"):
            assert not self.pending[e], e
        ops = self.ops
        with self.nc.Block() as block:
            @block.tensor
            def _(e):
                for f in ops["pe"]:
                    f(e)

            @block.vector
            def _(e):
                for f in ops["dve"]:
                    f(e)

            @block.scalar
            def _(e):
                for f in ops["act"]:
                    f(e)

            @block.gpsimd
            def _(e):
                for f in ops["pool"]:
                    f(e)

            @block.sync
            def _(e):
                for f in ops["sp"]:
                    f(e)


class Cfg:
    def __init__(self, S=8192, T=1024, layers=4, n_experts=N_EXPERTS, final_plain=True):
        self.S, self.T, self.L = S, T, layers
        self.NT = S // T
        self.NG = T // 512
        self.NE = n_experts
        self.layer_is_moe = [(l % 2 == 1) for l in range(layers)]
        self.n_dense = sum(1 for m in self.layer_is_moe if not m)
        self.n_moe = sum(1 for m in self.layer_is_moe if m)
        self.stage = 99


def par_layout(L, n_moe):
    off = {}
    o = 0
    for name, n in (("cw", L * 4 * CONV_K), ("cb", L * 4), ("clg", L * 4), ("clb", L * 4), ("gng", L * 4),
                    ("lnp", 4 * L * 8), ("rout", max(n_moe, 1) * 8 * 8)):
        off[name] = o
        o += n
    off["_n"] = o
    return off


C_MASK, C_QD, C_CDS, C_KD, C_ID, C_ONE, C_EPS = 0, 512, 640, 642, 898, 1026, 1154
NCF = 1156


def build_program(cfg):
    nc = bass.Bass("TRN2", target_bir_lowering=False)
    S_, T, L, NT, NG, NE = cfg.S, cfg.T, cfg.L, cfg.NT, cfg.NG, cfg.NE
    NSUB = T // 128
    PO = par_layout(L, cfg.n_moe)

    def din(name, shape):
        return nc.dram_tensor(name, list(shape), F32, kind="ExternalInput").ap()

    x_d = din("x", [S_, D_MODEL])
    win_d = din("win", [L, 6, 128, 4096])
    wout_d = din("wout", [L, 2, 128, 4096])
    dw_d = [din("dw%d" % k, [max(cfg.n_dense, 1) * NSLAB, 128, 4096]) for k in (1, 3, 2)]
    mw_d = [din("mw%d" % k, [max(cfg.n_moe, 1) * NE * NSLAB, 128, 4096]) for k in (1, 3, 2)]
    par_d = din("par", [128, PO["_n"]])
    rotf_d = din("rotf", [2, 128, S_])
    rott_d = din("rott", [2, S_, 64])
    cf_d = din("constf", [128, NCF])
    cb_d = din("constb", [128, 384])
    out_d = nc.dram_tensor("out", [S_, D_MODEL], F32, kind="ExternalOutput").ap()

    S = Sched(nc)
    sb = nc.alloc_sbuf_tensor
    XT = sb("XT", [128, 8, T], F32)
    HT = sb("HT", [128, 8, T], BF16)
    W = sb("W", [128, 8, 4096], BF16)
    CF = sb("CF", [128, NCF], F32)
    CB = sb("CBb", [128, 384], BF16)
    PAR = sb("PAR", [128, PO["_n"]], F32)
    PARA = sb("PARA", [128, 4 * L * 8], F32)
    ST = sb("ST", [128, L, 2, 128], F32)
    HIST = sb("HIST", [128, L, 4, 30], BF16)
    U = sb("U", [128, 4, 542], BF16)
    DR = sb("DR", [128, 8, 128], BF16)
    ACC = sb("ACC", [128, 4, 512], F32)
    MG = sb("MG", [128, 2, 4, 512], BF16)
    QR = sb("QR", [128, 2, 512], BF16)
    QT = sb("QT", [128, 2, 512], BF16)
    KZ = sb("KZ", [128, 2, 2, 512], BF16)
    KTZ = sb("KTZ", [128, 2, 4, 256], BF16)
    VT = sb("VT", [128, 4, 512], BF16)
    NSTB = 4
    STB = sb("STB", [128, NSTB, 4, 128], BF16)
    ROTFB = sb("ROTFB", [128, 2, 512], F32)
    ROTTB = sb("ROTTB", [128, 2, 4, 64], F32)
    NF, NB = 6, 6
    TMPF = sb("TMPF", [128, NF, 512], F32)
    TMPB = sb("TMPB", [128, NB, 512], BF16)
    RS = sb("RS", [128, 512], F32)
    NM = sb("NM", [128, 512], F32)
    GBE = sb("GBE", [128, T], F32)
    GATE = sb("GATE", [128, NSUB, 8], F32)
    SM = sb("SM", [128, 16, 32], F32)
    NPS = 6
    PS = [nc.alloc_psum_tensor("ps%d" % i, [128, 512], F32) for i in range(NPS)]
    PSM = nc.alloc_psum_tensor("psm", [128, 512], F32)
    PSQ = nc.alloc_psum_tensor("psq", [128, 512], F32)

    B_XT = [[Buf("xt%d_%d" % (g, k)) for k in range(8)] for g in range(NG)]
    B_HT = [Buf("ht%d" % g) for g in range(NG)]
    B_W = [Buf("w%d" % u) for u in range(8)]
    B_CF, B_CB, B_PAR, B_PARA = Buf("cf"), Buf("cb"), Buf("par"), Buf("para")
    B_ST = [Buf("st%d" % l) for l in range(L)]
    B_HIST = [[Buf("hist%d_%d" % (l, c)) for c in range(4)] for l in range(L)]
    B_U = [Buf("u%d" % i) for i in range(4)]
    B_DR = [Buf("dr%d" % i) for i in range(8)]
    B_ACC = [Buf("acc%d" % c) for c in range(4)]
    B_MG = [Buf("mg0"), Buf("mg1")]
    B_QR = [Buf("qr0"), Buf("qr1")]
    B_QT = [Buf("qt0"), Buf("qt1")]
    B_KZ = [Buf("kz0"), Buf("kz1")]
    B_KTZ = [Buf("ktz%d" % s) for s in range(4)]
    B_VT = [Buf("vt%d" % s) for s in range(4)]
    B_STB = [Buf("stb%d" % i) for i in range(NSTB)]
    B_ROTF, B_ROTT = Buf("rotf"), Buf("rott")
    B_TF = [Buf("tf%d" % i) for i in range(NF)]
    B_TB = [Buf("tb%d" % i) for i in range(NB)]
    B_RS, B_NM = Buf("rs"), Buf("nm")
    B_GBE = Buf("gbe")
    B_GATE = [Buf("gate%d" % g) for g in range(NG)]
    B_SM = Buf("sm")
    B_PS = [Buf("ps%d" % i, True) for i in range(NPS)]
    B_PSM, B_PSQ = Buf("psm", True), Buf("psq", True)
    B_OUT = Buf("out")

    ctr = {"ps": 0, "tf": 0, "tb": 0, "stb": 0, "u": 0, "dr": 0}

    def bank():
        i = ctr["ps"] % NPS
        ctr["ps"] += 1
        return PS[i], B_PS[i]

    def tf():
        i = ctr["tf"] % NF
        ctr["tf"] += 1
        return TMPF[:, i, :], B_TF[i]

    def tb():
        i = ctr["tb"] % NB
        ctr["tb"] += 1
        return TMPB[:, i, :], B_TB[i]

    def mm(out, lhsT, rhs, start, stop, reads, writes, inc):
        S.op("pe", lambda e: e.matmul(out, lhsT, rhs, start=start, stop=stop), reads, writes, inc)

    def act(out, in_, func, reads, writes, **kw):
        S.op("act", lambda e: e.activation(out=out, in_=in_, func=func, **kw), reads, writes)

    def tt(eng, out, in0, in1, op, reads, writes):
        S.op(eng, lambda e: e.tensor_tensor(out=out, in0=in0, in1=in1, op=op), reads, writes)

    def stt(eng, out, in0, scalar, in1, op0, op1, reads, writes):
        S.op(eng, lambda e: e.scalar_tensor_tensor(out=out, in0=in0, scalar=scalar, in1=in1, op0=op0, op1=op1), reads, writes)

    def ts(eng, out, in0, s1, s2, op0, op1, reads, writes):
        if s2 is None:
            S.op(eng, lambda e: e.tensor_scalar(out=out, in0=in0, scalar1=s1, scalar2=None, op0=op0), reads, writes)
        else:
            S.op(eng, lambda e: e.tensor_scalar(out=out, in0=in0, scalar1=s1, scalar2=s2, op0=op0, op1=op1), reads, writes)

    def cp(eng, out, in_, reads, writes):
        S.op(eng, lambda e: e.tensor_copy(out=out, in_=in_), reads, writes)

    def pcol(name, idx):
        o = PO[name] + idx
        return PAR[:, o:o + 1]

    EPS = CF[:, C_EPS:C_EPS + 1]
    IDENT = CF[:, C_ID:C_ID + 128]
    ONESF = CF[:, C_ONE:C_ONE + 128]
    MASK = CF[:, C_MASK:C_MASK + 512]
    ONES128, ONES512, ONES1024 = CB[:, 0:128], CB[:, 128:256], CB[:, 256:384]

    def Wk(u, n):
        return W[:, u, :].rearrange("p (a b) -> p a b", b=n)

    S.dma("sp", CF[:], cf_d, writes=[B_CF])
    S.dma("sp", PAR[:], par_d, writes=[B_PAR])
    S.dma("pool", CB[:], cb_d, writes=[B_CB])
    DBG = os.environ.get("KDBG", "")
    if "m" not in DBG:
      S.op("pool", lambda e: e.memset(ST[:], 0.0), writes=B_ST)
    if "m" not in DBG:
      S.op("pool", lambda e: e.memset(HIST[:], 0.0), writes=[b for bl in B_HIST for b in bl])
    if "m" not in DBG:
      S.op("pool", lambda e: e.memset(KZ[:], 0.0), writes=B_KZ)
    if "m" not in DBG:
      S.op("pool", lambda e: e.memset(KTZ[:], 0.0), writes=B_KTZ)
    if "m" not in DBG:
      S.op("pool", lambda e: e.memset(STB[:], 0.0), writes=B_STB)
    lnp0 = PO["lnp"]
    ts("dve", PARA[:], PAR[:, lnp0:lnp0 + 4 * L * 8], float(ALPHA), None, ALU.mult, None, [B_PAR], [B_PARA])

    def lnp(which, l, kc, scaled):
        i = (which * L + l) * 8 + kc
        return PARA[:, i:i + 1] if scaled else PAR[:, lnp0 + i:lnp0 + i + 1]

    def finish_stats():
        S.op("act", lambda e: e.activation(out=RS[:], in_=PSM[:], func=AF.Square), [B_PSM], [B_RS])
        tt("dve", RS[:], PSQ[:], RS[:], ALU.subtract, [B_PSQ, B_RS], [B_RS])
        S.op("act", lambda e: e.activation(out=RS[:], in_=RS[:], func=AF.Sqrt, bias=EPS, scale=1.0), [B_RS, B_CF], [B_RS])
        S.op("dve", lambda e: e.reciprocal(out=RS[:], in_=RS[:]), [B_RS], [B_RS])
        stt("dve", NM[:], PSM[:], -1.0, RS[:], ALU.mult, ALU.mult, [B_PSM, B_RS], [B_NM])

    def layernorm(g, which, l, write_ht, scaled):
        cs = slice(g * 512, (g + 1) * 512)
        for kc in range(8):
            xb, bxb = tb()
            act(xb, XT[:, kc, cs], AF.Copy, [B_XT[g][kc]], [bxb])
            mm(PSM[:], ONES1024, xb, kc == 0, kc == 7, [bxb, B_CB], [B_PSM], True)
            xq, bxq = tb()
            act(xq, XT[:, kc, cs], AF.Square, [B_XT[g][kc]], [bxq])
            mm(PSQ[:], ONES1024, xq, kc == 0, kc == 7, [bxq, B_CB], [B_PSQ], True)
        finish_stats()
        for kc0 in range(0, 8, 2):
          tl = [tf(), tf()]
          for i_ in range(2):
            tt("dve", tl[i_][0], XT[:, kc0 + i_, cs], RS[:], ALU.mult, [B_XT[g][kc0 + i_], B_RS], [tl[i_][1]])
          for i_ in range(2):
            tt("dve", tl[i_][0], tl[i_][0], NM[:], ALU.add, [tl[i_][1], B_NM], [tl[i_][1]])
          for i_ in range(2):
            kc = kc0 + i_
            t, bt = tl[i_]
            if write_ht:
                act(HT[:, kc, cs], t, AF.Identity, [bt, B_PAR], [B_HT[g]], scale=lnp(which, l, kc, False), bias=lnp(which + 1, l, kc, False))
            act(XT[:, kc, cs], t, AF.Identity, [bt, B_PAR, B_PARA], [B_XT[g][kc]],
                scale=lnp(which, l, kc, scaled), bias=lnp(which + 1, l, kc, scaled))

    def load_tile(ti):
        for s in range(NSUB):
            g, s4 = s // 4, s % 4
            io = ACC[:, 2 * (s % 2):2 * (s % 2) + 2, :].rearrange("p a b -> p (a b)")
            bio = [B_ACC[2 * (s % 2)], B_ACC[2 * (s % 2) + 1]]
            r0 = ti * T + s * 128
            S.dma("sp", io, x_d[r0:r0 + 128, :], writes=bio)
            for kcb in range(2):
                pb, bpb = bank()
                for k4 in range(4):
                    kc = kcb * 4 + k4
                    S.op("pe", lambda e, pb=pb, k4=k4, kc=kc, io=io: e.transpose(pb[:, k4 * 128:(k4 + 1) * 128], io[:, kc * 128:(kc + 1) * 128], IDENT),
                         bio + [B_CF], [bpb], inc=(k4 == 3))
                pv = pb[:].rearrange("p (a b) -> p a b", b=128)
                ts_ = slice(s * 128, (s + 1) * 128)
                if "a" not in DBG:
                    act(HT[:, kcb * 4:(kcb + 1) * 4, ts_], pv, AF.Copy, [bpb], [B_HT[g]])
                if "v" not in DBG:
                    ts("dve", XT[:, kcb * 4:(kcb + 1) * 4, ts_], pv, float(ALPHA), None, ALU.mult, None, [bpb], B_XT[g][kcb * 4:(kcb + 1) * 4])

    def store_tile(ti):
        for s in range(NSUB):
            g = s // 4
            io = ACC[:, 2 * (s % 2):2 * (s % 2) + 2, :]
            bio = [B_ACC[2 * (s % 2)], B_ACC[2 * (s % 2) + 1]]
            for kcb in range(2):
                pb, bpb = bank()
                for k4 in range(4):
                    kc = kcb * 4 + k4
                    S.op("pe", lambda e, pb=pb, k4=k4, kc=kc, s=s: e.transpose(pb[:, k4 * 128:(k4 + 1) * 128], XT[:, kc, s * 128:(s + 1) * 128], IDENT),
                         [B_XT[g][kc], B_CF], [bpb], inc=(k4 == 3))
                if kcb == 0:
                    act(io[:, 0, :], pb[:], AF.Copy, [bpb], [bio[0]])
                else:
                    cp("dve", io[:, 1, :], pb[:], [bpb], [bio[1]])
            r0 = ti * T + s * 128
            S.dma("sp", out_d[r0:r0 + 128, :], io.rearrange("p a b -> p (a b)"), reads=bio, writes=[B_OUT])

    MU = {}
    SL = {"E": 0, "F": 1}

    def load_mixer_weights(l):
        E, F = SL["E"], SL["F"]
        MU.update({"q": 6, "k": 7, "ga": 3 * E, "gb": 3 * E + 1, "g": 3 * E + 2, "v": 3 * F, "o0": 3 * F + 1, "o1": 3 * F + 2})
        for nm_, j in (("q", 2), ("k", 3), ("ga", 0), ("gb", 1), ("g", 5), ("v", 4)):
            S.dma("pool", W[:, MU[nm_], :], win_d[l, j], writes=[B_W[MU[nm_]]])
        for j in range(2):
            S.dma("pool", W[:, MU["o%d" % j], :], wout_d[l, j], writes=[B_W[MU["o%d" % j]]])

    def load_slab(wd, idx, slot):
        for k in range(3):
            S.dma("pool", W[:, 3 * slot + k, :], wd[k][idx], writes=[B_W[3 * slot + k]])

    def mixer_group(ti, l, g):
        cs = slice(g * 512, (g + 1) * 512)
        tok0 = ti * T + g * 512
        bht = B_HT[g]
        S.dma("sp", ROTFB[:], rotf_d[:, :, tok0:tok0 + 512].rearrange("c p t -> p c t"), writes=[B_ROTF])
        for c_ in range(2):
            S.dma("sp", ROTTB[:, c_, :, :], rott_d[c_, tok0:tok0 + 512, :].rearrange("(s p) d -> p s d", p=128), writes=[B_ROTT])
        W0, W1, W2, W3, W4, W5 = (Wk(MU[n_], 512) for n_ in ("ga", "gb", "q", "k", "v", "g"))
        W6, W7 = Wk(MU["o0"], 1024), Wk(MU["o1"], 1024)
        BW0, BW1, BW2, BW3, BW4, BW5, BW6, BW7 = (B_W[MU[n_]] for n_ in ("ga", "gb", "q", "k", "v", "g", "o0", "o1"))

        def proj_fm(wv, bw, c0):
            pb, bpb = bank()
            for kc in range(8):
                mm(pb[:], wv[:, kc, c0:c0 + 128], HT[:, kc, cs], kc == 0, kc == 7, [bw, bht], [bpb], kc == 7)
            return pb, bpb

        if cfg.stage < 1:
            return
        for hp in range(2):
            pq, bq = proj_fm(W2, BW2, hp * 128)
            pqp, bqp = proj_fm(W2, BW2, 256 + hp * 128)
            f1, b1 = tf()
            f2, b2 = tf()
            tt("dve", f1, pq[:], ROTFB[:, 0, :], ALU.mult, [bq, B_ROTF], [b1])
            tt("dve", f2, pqp[:], ROTFB[:, 1, :], ALU.mult, [bqp, B_ROTF], [b2])
            tt("dve", f1, f1, f2, ALU.add, [b1, b2], [b1])
            act(QR[:, hp, :], f1, AF.Copy, [b1], [B_QR[hp]])
            qd = CF[:, C_QD + hp * 64:C_QD + (hp + 1) * 64].unsqueeze(1).broadcast_to([128, 8, 64])
            tt("dve", QT[:, hp, :].rearrange("p (c i) -> p c i", i=64), f1.rearrange("p (c i) -> p c i", i=64), qd, ALU.mult,
               [b1, B_CF], [B_QT[hp]])
        for hp in range(2):
            pk, bk = proj_fm(W3, BW3, hp * 128)
            pkp, bkp = proj_fm(W3, BW3, 256 + hp * 128)
            f1, b1 = tf()
            f2, b2 = tf()
            tt("dve", f1, pk[:], ROTFB[:, 0, :], ALU.mult, [bk, B_ROTF], [b1])
            tt("dve", f2, pkp[:], ROTFB[:, 1, :], ALU.mult, [bkp, B_ROTF], [b2])
            for hh in range(2):
                rows = slice(hh * 64, (hh + 1) * 64)
                tt("dve", KZ[rows, hp, hh, :], f1[rows], f2[rows], ALU.add, [b1, b2], [B_KZ[hp]])
        if cfg.stage < 2:
            return
        for c in range(4):
            pa, ba = proj_fm(W0, BW0, c * 128)
            pbk, bb = proj_fm(W1, BW1, c * 128)
            sg, bsg = tf()
            act(sg, pbk[:], AF.Sigmoid, [bb], [bsg])
            Uc, bu = U[:, c, :], B_U[c]
            cp("pool", Uc[:, 0:30], HIST[:, l, c, :], [B_HIST[l][c]], [bu])
            tt("dve", Uc[:, 30:542], pa[:], sg, ALU.mult, [ba, bsg], [bu])
            cp("pool", HIST[:, l, c, :], Uc[:, 512:542], [bu], [B_HIST[l][c]])
        for c in range(4):
            pcv, bcv = bank()
            for j in range(CONV_K):
                slot = ctr["dr"] % 8
                ctr["dr"] += 1
                ts("pool", DR[:, slot, :], IDENT, pcol("cw", (l * 4 + c) * CONV_K + j), None, ALU.mult, None, [B_CF, B_PAR], [B_DR[slot]])
                mm(pcv[:], DR[:, slot, :], U[:, c, j:j + 512], j == 0, j == CONV_K - 1, [B_DR[slot], B_U[c]], [bcv], True)
            acc, bacc = ACC[:, c, :], B_ACC[c]
            act(acc, pcv[:], AF.Identity, [bcv, B_PAR], [bacc], bias=pcol("cb", l * 4 + c), scale=1.0)
            xb, bxb = tb()
            act(xb, acc, AF.Copy, [bacc], [bxb])
            mm(PSM[:], ONES512, xb, c == 0, c == 3, [bxb, B_CB], [B_PSM], True)
            xq, bxq = tb()
            act(xq, acc, AF.Square, [bacc], [bxq])
            mm(PSQ[:], ONES512, xq, c == 0, c == 3, [bxq, B_CB], [B_PSQ], True)
        finish_stats()
        for c in range(4):
            acc, bacc = ACC[:, c, :], B_ACC[c]
            tt("dve", acc, acc, RS[:], ALU.mult, [bacc, B_RS], [bacc])
            tt("dve", acc, acc, NM[:], ALU.add, [bacc, B_NM], [bacc])
            act(MG[:, 0, c, :], acc, AF.Silu, [bacc, B_PAR], [B_MG[0]], scale=pcol("clg", l * 4 + c), bias=pcol("clb", l * 4 + c))
        if cfg.stage < 3:
            return
        for h in range(4):
            pg, bg = proj_fm(W5, BW5, h * 128)
            act(MG[:, 1, h, :], pg[:], AF.Silu, [bg], [B_MG[1]])
        if cfg.stage < 4:
            return
        for s in range(4):
            tsl = slice(g * 512 + s * 128, g * 512 + (s + 1) * 128)
            pkt, bkt = bank()
            for kc in range(8):
                mm(pkt[:], HT[:, kc, tsl], W3[:, kc, :], kc == 0, kc == 7, [BW3, bht], [bkt], kc == 7)
            pv, bv = bank()
            for kc in range(8):
                mm(pv[:], HT[:, kc, tsl], W4[:, kc, :], kc == 0, kc == 7, [BW4, bht], [bv], kc == 7)
            act(VT[:, s, :], pv[:], AF.Copy, [bv], [B_VT[s]])
            f1, b1 = tf()
            f2, b2 = tf()
            cosb = ROTTB[:, 0, s, :].unsqueeze(1).broadcast_to([128, 4, 64])
            sinb = ROTTB[:, 1, s, :].unsqueeze(1).broadcast_to([128, 4, 64])
            tt("dve", f1[:, 0:256].rearrange("p (h d) -> p h d", d=64), pkt[:, 0:256].rearrange("p (h d) -> p h d", d=64), cosb, ALU.mult, [bkt, B_ROTT], [b1])
            tt("dve", f2[:, 0:256].rearrange("p (h d) -> p h d", d=64), pkt[:, 256:512].rearrange("p (h d) -> p h d", d=64), sinb, ALU.mult, [bkt, B_ROTT], [b2])
            tt("dve", f1[:, 0:256], f1[:, 0:256], f2[:, 0:256], ALU.add, [b1, b2], [b1])
            for c in range(2):
                rows = slice(c * 64, (c + 1) * 64)
                tt("dve", KTZ[rows, c, s, :], f1[rows, 0:256], CF[rows, C_KD:C_KD + 256], ALU.mult, [b1, B_CF], [B_KTZ[s]])
        if cfg.stage < 5:
            return
        for s in range(4):
            ssl = slice(s * 128, (s + 1) * 128)
            slots = []
            for c in range(2):
                pkv, bkv = bank()
                for hp in range(2):
                    mm(pkv[:, hp * 256:(hp + 1) * 256], KTZ[:, c, s, hp * 128:(hp + 1) * 128], VT[:, s, hp * 256:(hp + 1) * 256],
                       True, True, [B_KTZ[s], B_VT[s]], [bkv], hp == 1)
                slot = ctr["stb"] % NSTB
                ctr["stb"] += 1
                slots.append(slot)
                for h in range(4):
                    hp, hh = h // 2, h % 2
                    rows = slice(hh * 64, (hh + 1) * 64)
                    act(STB[rows, slot, h, :], ST[rows, l, hp, :], AF.Copy, [B_ST[l]], [B_STB[slot]])
                for h in range(4):
                    hp, hh = h // 2, h % 2
                    rows = slice(hh * 64, (hh + 1) * 64)
                    stt("dve", ST[rows, l, hp, :], ST[rows, l, hp, :], CF[rows, C_CDS + hp:C_CDS + hp + 1],
                        pkv[rows, hp * 256 + hh * 128:hp * 256 + (hh + 1) * 128], ALU.mult, ALU.add, [B_ST[l], B_CF, bkv], [B_ST[l]])
            psc, bsc = bank()
            for h in range(4):
                hp, hh = h // 2, h % 2
                mm(psc[:, h * 128:(h + 1) * 128], KZ[:, hp, hh, ssl], QR[:, hp, ssl], True, True, [B_KZ[hp], B_QR[hp]], [bsc], h == 3)
            smt, bsm = tb()
            tt("dve", smt, psc[:], MASK, ALU.mult, [bsc, B_CF], [bsm])
            po, bo = bank()
            for h in range(4):
                hp = h // 2
                mm(po[:, h * 128:(h + 1) * 128], VT[:, s, h * 128:(h + 1) * 128], smt[:, h * 128:(h + 1) * 128], True, False,
                   [B_VT[s], bsm], [bo], False)
                for c in range(2):
                    mm(po[:, h * 128 + c * 64:h * 128 + (c + 1) * 64], STB[:, slots[c], h, :], QT[:, hp, s * 128 + c * 64:s * 128 + (c + 1) * 64],
                       False, True, [B_STB[slots[c]], B_QT[hp]], [bo], (h == 3 and c == 1))
            ob, bob = tb()
            act(ob, po[:], AF.Copy, [bo], [bob])
            mm(PSM[:], ONES128, ob, True, True, [bob, B_CB], [B_PSM], True)
            oq, boq = tb()
            act(oq, po[:], AF.Square, [bo], [boq])
            mm(PSQ[:], ONES128, oq, True, True, [boq, B_CB], [B_PSQ], True)
            finish_stats()
            y, by = tf()
            tt("dve", y, po[:], RS[:], ALU.mult, [bo, B_RS], [by])
            tt("dve", y, y, NM[:], ALU.add, [by, B_NM], [by])
            gng = PAR[:, PO["gng"] + l * 4:PO["gng"] + l * 4 + 4].unsqueeze(2).broadcast_to([128, 4, 128])
            y3 = y.rearrange("p (h i) -> p h i", i=128)
            tt("dve", y3, y3, gng, ALU.mult, [by, B_PAR], [by])
            mgv = MG[:, 1, :, g * 0 + s * 128:g * 0 + (s + 1) * 128]
            tt("dve", mgv, y3, mgv, ALU.mult, [by, B_MG[1]], [B_MG[1]])
        if cfg.stage < 6:
            return
        for dc in range(8):
            py, bpy = bank()
            for mc in range(8):
                wv, bw = (W6, BW6) if mc < 4 else (W7, BW7)
                mm(py[:], wv[:, mc % 4, dc * 128:(dc + 1) * 128], MG[:, mc // 4, mc % 4, :], mc == 0, mc == 7, [bw, B_MG[mc // 4]], [bpy], mc == 7)
            tt("dve", XT[:, dc, cs], py[:], XT[:, dc, cs], ALU.add, [bpy, B_XT[g][dc]], [B_XT[g][dc]])
        layernorm(g, 0, l, True, True)

    def routing_group(li, g):
        plg, blg = bank()
        for s in range(4):
            tsl = slice(g * 512 + s * 128, g * 512 + (s + 1) * 128)
            for kc in range(8):
                ro = PO["rout"] + (li * 8 + kc) * 8
                mm(plg[:, s * 8:(s + 1) * 8], XT[:, kc, tsl], PAR[:, ro:ro + 8], kc == 0, kc == 7, [B_XT[g][kc], B_PAR], [blg], (s == 3 and kc == 7))
        lg = SM[:, 0, :].rearrange("p (s e) -> p s e", e=8)
        eq1 = SM[:, 1, :].rearrange("p (s e) -> p s e", e=8)
        l2 = SM[:, 2, :].rearrange("p (s e) -> p s e", e=8)
        eq2 = SM[:, 3, :].rearrange("p (s e) -> p s e", e=8)
        m1, m2, dd, ee, p1, p2 = (SM[:, 4 + i, 0:4] for i in range(6))
        bs = [B_SM]
        cp("dve", SM[:, 0, :], plg[:, 0:32], [blg], bs)
        S.op("dve", lambda e: e.tensor_reduce(out=m1, in_=lg, axis=AX.X, op=ALU.max), bs, bs)
        tt("dve", eq1, lg, m1.unsqueeze(2).broadcast_to([128, 4, 8]), ALU.is_equal, bs, bs)
        stt("dve", l2, eq1, -1e30, lg, ALU.mult, ALU.add, bs, bs)
        S.op("dve", lambda e: e.tensor_reduce(out=m2, in_=l2, axis=AX.X, op=ALU.max), bs, bs)
        tt("dve", eq2, l2, m2.unsqueeze(2).broadcast_to([128, 4, 8]), ALU.is_equal, bs, bs)
        tt("dve", dd, m2, m1, ALU.subtract, bs, bs)
        act(ee, dd, AF.Exp, bs, bs, scale=float(1.0 / ALPHA))
        ts("dve", p1, ee, 1.0, None, ALU.add, None, bs, bs)
        S.op("dve", lambda e: e.reciprocal(out=p1, in_=p1), bs, bs)
        tt("dve", p2, ee, p1, ALU.mult, bs, bs)
        tt("dve", eq1, eq1, p1.unsqueeze(2).broadcast_to([128, 4, 8]), ALU.mult, bs, bs)
        tt("dve", eq2, eq2, p2.unsqueeze(2).broadcast_to([128, 4, 8]), ALU.mult, bs, bs)
        tt("dve", GATE[:, g * 4:(g + 1) * 4, :], eq1, eq2, ALU.add, bs, [B_GATE[g]])

    def build_gbe(ex):
        for g in range(NG):
            pb, bpb = bank()
            r, br = tf()
            for s in range(4):
                ts("dve", r[:, s * 128:(s + 1) * 128], IDENT, GATE[:, g * 4 + s, ex:ex + 1], None, ALU.mult, None, [B_CF, B_GATE[g]], [br])
            for s in range(4):
                mm(pb[:, s * 128:(s + 1) * 128], ONESF, r[:, s * 128:(s + 1) * 128], True, True, [br, B_CF], [bpb], s == 3)
            act(GBE[:, g * 512:(g + 1) * 512], pb[:], AF.Copy, [bpb], [B_GBE])

    def ffn_tile(l):
        moe = cfg.layer_is_moe[l]
        li = sum(1 for m in cfg.layer_is_moe[:l] if m == moe)
        wd = mw_d if moe else dw_d
        nex = NE if moe else 1
        slabs = [(ex, sl) for ex in range(nex) for sl in range(NSLAB)]
        base = li * nex * NSLAB
        E_, F_ = SL["E"], SL["F"]
        slot_of = lambda i: (E_ if i % 2 == 0 else F_)
        load_slab(wd, base + 0, slot_of(0))
        if len(slabs) > 1:
            load_slab(wd, base + 1, slot_of(1))
        items = [(i, g) for i in range(len(slabs)) for g in range(NG)]

        def phase_a(i, g, gi):
            ex, sl = slabs[i]
            slot = slot_of(i)
            cs = slice(g * 512, (g + 1) * 512)
            if moe and sl == 0 and g == 0:
                build_gbe(ex)
            W1s, W3s = Wk(3 * slot, 512), Wk(3 * slot + 1, 512)
            for fc in range(4):
                pa, ba = bank()
                for kc in range(8):
                    mm(pa[:], W1s[:, kc, fc * 128:(fc + 1) * 128], HT[:, kc, cs], kc == 0, kc == 7, [B_W[3 * slot], B_HT[g]], [ba], kc == 7)
                pc, bc = bank()
                for kc in range(8):
                    mm(pc[:], W3s[:, kc, fc * 128:(fc + 1) * 128], HT[:, kc, cs], kc == 0, kc == 7, [B_W[3 * slot + 1], B_HT[g]], [bc], kc == 7)
                sa, bsa = tf()
                act(sa, pa[:], AF.Silu, [ba], [bsa])
                if moe:
                    tt("dve", sa, pc[:], sa, ALU.mult, [bc, bsa], [bsa])
                    tt("dve", MG[:, gi % 2, fc, :], sa, GBE[:, cs], ALU.mult, [bsa, B_GBE], [B_MG[gi % 2]])
                else:
                    tt("dve", MG[:, gi % 2, fc, :], pc[:], sa, ALU.mult, [bc, bsa], [B_MG[gi % 2]])

        def phase_b(i, g, gi):
            slot = slot_of(i)
            cs = slice(g * 512, (g + 1) * 512)
            W2s = Wk(3 * slot + 2, 1024)
            for dc in range(8):
                py, bpy = bank()
                for fc in range(4):
                    mm(py[:], W2s[:, fc, dc * 128:(dc + 1) * 128], MG[:, gi % 2, fc, :], fc == 0, fc == 3, [B_W[3 * slot + 2], B_MG[gi % 2]], [bpy], fc == 3)
                tt("dve", XT[:, dc, cs], py[:], XT[:, dc, cs], ALU.add, [bpy, B_XT[g][dc]], [B_XT[g][dc]])
            if g == NG - 1 and i + 2 < len(slabs):
                load_slab(wd, base + i + 2, slot)

        for gi, (i, g) in enumerate(items):
            phase_a(i, g, gi)
            if gi > 0:
                pi, pg = items[gi - 1]
                phase_b(pi, pg, gi - 1)
        pi, pg = items[-1]
        phase_b(pi, pg, len(items) - 1)
        SL["E"], SL["F"] = slot_of(len(slabs) - 2), slot_of(len(slabs) - 1)

    for ti in range(NT):
        if "l" not in DBG:
            load_tile(ti)
        for l in range(L):
            if cfg.stage < -1:
                continue
            load_mixer_weights(l)
            if cfg.stage < 0:
                continue
            for g in range(NG):
                mixer_group(ti, l, g)
                if cfg.layer_is_moe[l]:
                    li = sum(1 for m in cfg.layer_is_moe[:l] if m)
                    routing_group(li, g)
            if cfg.stage < 7:
                continue
            ffn_tile(l)
            last = (l == L - 1)
            for g in range(NG):
                layernorm(g, 2, l, not last, not last)
        if "s" not in DBG:
            store_tile(ti)
    S.wait_all("sp", [B_OUT])
    S.emit()
    return nc, S


def host_constants(S_):
    half = RET_DK // 2
    inv_freq = (np.float32(10000.0) ** (-np.arange(half, dtype=np.float32) / np.float32(half))).astype(np.float32)
    pos = np.arange(S_, dtype=np.float32)
    ang = (pos[:, None] * inv_freq[None, :]).astype(np.float32)
    cos, sin = np.cos(ang).astype(np.float32), np.sin(ang).astype(np.float32)
    cos64 = np.concatenate([cos, cos], axis=1)
    sin64 = np.concatenate([-sin, sin], axis=1)
    rott = np.ascontiguousarray(np.stack([cos64, sin64], axis=0))
    rotf = np.ascontiguousarray(np.stack([np.tile(cos64.T, (2, 1)), np.tile(sin64.T, (2, 1))], axis=0))
    h = np.arange(RET_HEADS, dtype=np.float32)
    log_gamma = np.log1p(-(np.float32(2.0) ** (-5.0 - h))).astype(np.float32)
    idx = np.arange(CHUNK, dtype=np.float32)
    cf = np.zeros((128, NCF), np.float32)
    p = np.arange(128)
    for hh in range(RET_HEADS):
        d = np.abs(p[:, None] - p[None, :]).astype(np.float32)
        m = np.exp(log_gamma[hh] * d).astype(np.float32) * np.float32(RET_DK ** -0.5)
        m = m * ((p[:, None] // 64) == (p[None, :] // 64))
        cf[:, C_MASK + hh * 128:C_MASK + (hh + 1) * 128] = m
    for hp in range(2):
        for hh in range(2):
            cf[hh * 64:(hh + 1) * 64, C_QD + hp * 64:C_QD + (hp + 1) * 64] = np.exp(log_gamma[2 * hp + hh] * (idx + 1.0))[None, :]
            cf[hh * 64:(hh + 1) * 64, C_CDS + hp] = np.exp(log_gamma[2 * hp + hh] * np.float32(CHUNK))
    for hh in range(RET_HEADS):
        kd = np.exp(log_gamma[hh] * (CHUNK - 1.0 - (p % 64).astype(np.float32))).astype(np.float32) * np.float32(RET_DK ** -0.5)
        cf[:, C_KD + hh * 64:C_KD + (hh + 1) * 64] = kd[:, None]
    cf[:, C_ID:C_ID + 128] = np.eye(128, dtype=np.float32)
    cf[:, C_ONE:C_ONE + 128] = 1.0
    cf[:, C_EPS] = LN_EPS
    cb = np.zeros((128, 384), np.float32)
    cb[:, 0:128] = 1.0 / 128
    cb[:, 128:256] = 1.0 / 512
    cb[:, 256:384] = 1.0 / 1024
    return rotf, rott, cf, cb


def _slab_w13(w):
    lead = w.shape[:-2]
    w = w.reshape(lead + (8, 128, NSLAB, 512))
    n = len(lead)
    w = np.transpose(w, tuple(range(n)) + (n + 2, n + 1, n + 0, n + 3))
    return np.ascontiguousarray(w).reshape((-1, 128, 4096))


def _slab_w2(w):
    lead = w.shape[:-2]
    w = w.reshape(lead + (NSLAB, 4, 128, 1024))
    n = len(lead)
    w = np.transpose(w, tuple(range(n)) + (n + 0, n + 2, n + 1, n + 3))
    return np.ascontiguousarray(w).reshape((-1, 128, 4096))


def prepare_shared(cfg, w_in, w_out, conv_w, conv_b, conv_ln_g, conv_ln_b, ret_gn_g, ln1_g, ln1_b, ln2_g, ln2_b,
                   dense_w1, dense_w3, dense_w2, moe_router, moe_w1, moe_w3, moe_w2):
    L = cfg.L
    f = lambda a: np.asarray(a, dtype=np.float32)
    w_in, w_out = f(w_in)[:L], f(w_out)[:L]
    perm = np.concatenate([(np.arange(64) + 32) % 64 + 64 * hh for hh in range(RET_HEADS)])
    ga, gb = w_in[:, :, 0:512], w_in[:, :, 512:1024]
    q, k = w_in[:, :, 1024:1280], w_in[:, :, 1280:1536]
    v, g = w_in[:, :, 1536:2048], w_in[:, :, 2048:2560]
    ext = np.concatenate([ga, gb, q, q[:, :, perm], k, k[:, :, perm], v, g], axis=2)
    win = np.ascontiguousarray(ext.reshape(L, 8, 128, 6, 512).transpose(0, 3, 2, 1, 4)).reshape(L, 6, 128, 4096)
    wout = np.ascontiguousarray(w_out.reshape(L, 2, 4, 128, 1024).transpose(0, 1, 3, 2, 4)).reshape(L, 2, 128, 4096)
    nd, nm = max(cfg.n_dense, 1), max(cfg.n_moe, 1)
    dw = [_slab_w13(f(dense_w1)[:nd]), _slab_w13(f(dense_w3)[:nd]), _slab_w2(f(dense_w2)[:nd])]
    mw = [_slab_w13(f(moe_w1)[:nm, :cfg.NE]), _slab_w13(f(moe_w3)[:nm, :cfg.NE]), _slab_w2(f(moe_w2)[:nm, :cfg.NE])]
    PO = par_layout(L, cfg.n_moe)
    par = np.zeros((128, PO["_n"]), np.float32)
    cw = f(conv_w)[:L].reshape(L, CONV_K, 4, 128).transpose(3, 0, 2, 1)
    par[:, PO["cw"]:PO["cw"] + L * 4 * CONV_K] = cw.reshape(128, -1)
    for name, a in (("cb", conv_b), ("clg", conv_ln_g), ("clb", conv_ln_b), ("gng", ret_gn_g)):
        par[:, PO[name]:PO[name] + L * 4] = f(a)[:L].reshape(L, 4, 128).transpose(2, 0, 1).reshape(128, -1)
    lnp = np.stack([f(a)[:L].reshape(L, 8, 128).transpose(2, 0, 1) for a in (ln1_g, ln1_b, ln2_g, ln2_b)], axis=1)
    par[:, PO["lnp"]:PO["lnp"] + 4 * L * 8] = lnp.reshape(128, -1)
    if cfg.n_moe > 0:
        ro = f(moe_router)[:cfg.n_moe].reshape(cfg.n_moe, 8, 128, 8).transpose(2, 0, 1, 3)
        par[:, PO["rout"]:PO["rout"] + cfg.n_moe * 64] = ro.reshape(128, -1)
    rotf, rott, cf, cb = host_constants(cfg.S)
    sh = {"win": win, "wout": wout, "par": par, "rotf": rotf, "rott": rott, "constf": cf, "constb": cb}
    for i, kk in enumerate((1, 3, 2)):
        sh["dw%d" % kk] = dw[i]
        sh["mw%d" % kk] = mw[i]
    return sh


def run(cfg, x, **weights):
    nc, S = build_program(cfg)
    sh = prepare_shared(cfg, **weights)
    x = np.asarray(x, dtype=np.float32)
    ncore = x.shape[0]
    in_maps = []
    for c in range(ncore):
        m = dict(sh)
        m["x"] = np.ascontiguousarray(x[c])
        in_maps.append(m)
    res = run_bass_kernel_spmd(nc, in_maps, core_ids=list(range(ncore)))
    return np.stack([np.asarray(r["out"]) for r in res.results], axis=0).astype(np.float32)


def kernel(x, w_in, w_out, conv_w, conv_b, conv_ln_g, conv_ln_b, ret_gn_g, ln1_g, ln1_b, ln2_g, ln2_b,
           dense_w1, dense_w3, dense_w2, moe_router, moe_w1, moe_w3, moe_w2):
    cfg = Cfg(S=int(np.shape(x)[1]), T=1024, layers=DEPTH)
    return run(cfg, x, w_in=w_in, w_out=w_out, conv_w=conv_w, conv_b=conv_b, conv_ln_g=conv_ln_g, conv_ln_b=conv_ln_b,
               ret_gn_g=ret_gn_g, ln1_g=ln1_g, ln1_b=ln1_b, ln2_g=ln2_g, ln2_b=ln2_b, dense_w1=dense_w1, dense_w3=dense_w3,
               dense_w2=dense_w2, moe_router=moe_router, moe_w1=moe_w1, moe_w3=moe_w3, moe_w2=moe_w2)
```

```python
import os
import numpy as np
import concourse.bass as bass
import concourse.mybir as mybir
from concourse.bass_utils import run_bass_kernel_spmd

F32 = mybir.dt.float32
BF16 = mybir.dt.bfloat16
AF = mybir.ActivationFunctionType
ALU = mybir.AluOpType
AX = mybir.AxisListType

D_MODEL = 1024
DEPTH = 4
CHUNK = 64
CONV_CH = 512
CONV_K = 31
RET_HEADS = 4
RET_DK = 64
RET_DV = 128
D_FF = 3584
N_EXPERTS = 8
ALPHA = (2.0 * DEPTH) ** 0.25
LN_EPS = 1e-5
NSLAB = D_FF // 512
NOSTRICT = bool(os.environ.get("KNOSTRICT"))


class Buf:
    __slots__ = ("name", "w", "r", "excl")

    def __init__(self, name, excl=False):
        self.name = name
        self.w = None
        self.r = {}
        self.excl = excl


class Sched:
    ENGS = ("pe", "dve", "act", "pool", "sp")

    def __init__(self, nc, n_dma_sems=8):
        self.nc = nc
        self.ops = {e: [] for e in self.ENGS}
        self.cnt = {e: 0 for e in self.ENGS}
        self.seen = {e: {} for e in self.ENGS}
        self.sems = {}
        self.n_dma_sems = n_dma_sems
        self.dma_issue = {}
        self.pending = {e: False for e in self.ENGS}
        self.ninstr = 0
        self.log = {e: [] for e in self.ENGS}

    def sem(self, key):
        if key not in self.sems:
            nm = "s_" + "_".join(str(k) for k in (key if isinstance(key, tuple) else (key,)))
            self.sems[key] = self.nc.alloc_semaphore(nm)
        return self.sems[key]

    def _wait(self, eng, key, val):
        if val <= 0 or self.seen[eng].get(key, 0) >= val:
            return
        self.seen[eng][key] = val
        sem = self.sem(key)
        self.ops[eng].append(lambda e, sem=sem, val=val: e.wait_ge(sem, val))
        self.log[eng].append(("w", key, val))
        self.ninstr += 1

    def _deps(self, eng, reads, writes, skip_self):
        deps = {}
        for b in reads:
            if b.w is not None and deps.get(b.w[0], 0) < b.w[1]:
                deps[b.w[0]] = b.w[1]
            if b.excl:
                for k, v in b.r.items():
                    if k != eng and deps.get(k, 0) < v:
                        deps[k] = v
        for b in writes:
            if b.w is not None and deps.get(b.w[0], 0) < b.w[1]:
                deps[b.w[0]] = b.w[1]
            for k, v in b.r.items():
                if deps.get(k, 0) < v:
                    deps[k] = v
        for k, v in deps.items():
            if k == eng and skip_self:
                continue
            self._wait(eng, k, v)

    def _record(self, ev, reads, writes):
        for b in reads:
            if b.r.get(ev[0], 0) < ev[1]:
                b.r[ev[0]] = ev[1]
        for b in writes:
            b.w = ev
            b.r = {}

    def op(self, eng, fn, reads=(), writes=(), inc=True):
        self._deps(eng, reads, writes, skip_self=(eng == "pe" or NOSTRICT))
        if inc:
            self.cnt[eng] += 1
            ev = (eng, self.cnt[eng])
            sem = self.sem(eng)
            self.ops[eng].append(lambda e, fn=fn, sem=sem: fn(e).then_inc(sem, 1))
            self.log[eng].append(("i", eng, 1))
            self.pending[eng] = False
        else:
            ev = (eng, self.cnt[eng] + 1)
            self.ops[eng].append(lambda e, fn=fn: fn(e))
            self.pending[eng] = True
        self.ninstr += 1
        self._record(ev, reads, writes)
        return ev

    def dma(self, queue, out, in_, reads=(), writes=()):
        i = self.dma_issue.get(queue, 0)
        self.dma_issue[queue] = i + 1
        slot, use = i % self.n_dma_sems, i // self.n_dma_sems
        key = ("dma", queue, slot)
        self._wait(queue, key, 16 * use)
        self._deps(queue, reads, writes, skip_self=False)
        sem = self.sem(key)
        ev = (key, 16 * (use + 1))
        self.ops[queue].append(lambda e, out=out, in_=in_, sem=sem: e.dma_start(out=out, in_=in_).then_inc(sem, 16))
        self.log[queue].append(("i", key, 16))
        self.ninstr += 1
        self._record(ev, reads, writes)
        return ev

    def wait_all(self, eng, bufs):
        self._deps(eng, (), bufs, skip_self=False)

    def check_deadlock(self):
        val = {}
        pos = {e: 0 for e in self.ENGS}
        progress = True
        while progress:
            progress = False
            for e in self.ENGS:
                lg = self.log[e]
                while pos[e] < len(lg):
                    kind, key, v = lg[pos[e]]
                    if kind == "w":
                        if val.get(key, 0) < v:
                            break
                    else:
                        val[key] = val.get(key, 0) + v
                    pos[e] += 1
                    progress = True
        bad = {e: (pos[e], len(self.log[e]), self.log[e][pos[e]]) for e in self.ENGS if pos[e] < len(self.log[e])}
        return bad

    def emit(self):
        for e in ("pe", "dve", "act", "pool"):
            assert not self.pending[e], e
        ops = self.ops
        with self.nc.Block() as block:
            @block.tensor
            def _(e):
                for f in ops["pe"]:
                    f(e)

            @block.vector
            def _(e):
                for f in ops["dve"]:
                    f(e)

            @block.scalar
            def _(e):
                for f in ops["act"]:
                    f(e)

            @block.gpsimd
            def _(e):
                for f in ops["pool"]:
                    f(e)

            @block.sync
            def _(e):
                for f in ops["sp"]:
                    f(e)


class Cfg:
    def __init__(self, S=8192, T=1024, layers=4, n_experts=N_EXPERTS, final_plain=True):
        self.S, self.T, self.L = S, T, layers
        self.NT = S // T
        self.NG = T // 512
        self.NE = n_experts
        self.layer_is_moe = [(l % 2 == 1) for l in range(layers)]
        self.n_dense = sum(1 for m in self.layer_is_moe if not m)
        self.n_moe = sum(1 for m in self.layer_is_moe if m)
        self.stage = 99


def par_layout(L, n_moe):
    off = {}
    o = 0
    for name, n in (("cw", L * 4 * CONV_K), ("cb", L * 4), ("clg", L * 4), ("clb", L * 4), ("gng", L * 4),
                    ("lnp", 4 * L * 8), ("rout", max(n_moe, 1) * 8 * 8)):
        off[name] = o
        o += n
    off["_n"] = o
    return off


C_MASK, C_QD, C_CDS, C_KD, C_ID, C_ONE, C_EPS = 0, 512, 640, 642, 898, 1026, 1154
NCF = 1156


def build_program(cfg):
    nc = bass.Bass("TRN2", target_bir_lowering=False)
    S_, T, L, NT, NG, NE = cfg.S, cfg.T, cfg.L, cfg.NT, cfg.NG, cfg.NE
    NSUB = T // 128
    PO = par_layout(L, cfg.n_moe)

    def din(name, shape):
        return nc.dram_tensor(name, list(shape), F32, kind="ExternalInput").ap()

    x_d = din("x", [S_, D_MODEL])
    win_d = din("win", [L, 6, 128, 4096])
    wout_d = din("wout", [L, 2, 128, 4096])
    dw_d = [din("dw%d" % k, [max(cfg.n_dense, 1) * NSLAB, 128, 4096]) for k in (1, 3, 2)]
    mw_d = [din("mw%d" % k, [max(cfg.n_moe, 1) * NE * NSLAB, 128, 4096]) for k in (1, 3, 2)]
    par_d = din("par", [128, PO["_n"]])
    rotf_d = din("rotf", [2, 128, S_])
    rott_d = din("rott", [2, S_, 64])
    cf_d = din("constf", [128, NCF])
    cb_d = din("constb", [128, 384])
    out_d = nc.dram_tensor("out", [S_, D_MODEL], F32, kind="ExternalOutput").ap()

    S = Sched(nc)
    sb = nc.alloc_sbuf_tensor
    XT = sb("XT", [128, 8, T], F32)
    HT = sb("HT", [128, 8, T], BF16)
    W = sb("W", [128, 8, 4096], BF16)
    CF = sb("CF", [128, NCF], F32)
    CB = sb("CBb", [128, 384], BF16)
    PAR = sb("PAR", [128, PO["_n"]], F32)
    PARA = sb("PARA", [128, 4 * L * 8], F32)
    ST = sb("ST", [128, L, 2, 128], F32)
    HIST = sb("HIST", [128, L, 4, 30], BF16)
    U = sb("U", [128, 4, 542], BF16)
    NDR = 16
    DR = sb("DR", [128, NDR, 128], BF16)
    ACC = sb("ACC", [128, 4, 512], F32)
    MG = sb("MG", [128, 2, 4, 512], BF16)
    QR = sb("QR", [128, 2, 512], BF16)
    QT = sb("QT", [128, 2, 512], BF16)
    KZ = sb("KZ", [128, 2, 2, 512], BF16)
    KTZ = sb("KTZ", [128, 2, 4, 256], BF16)
    VT = sb("VT", [128, 4, 512], BF16)
    NSTB = 4
    STB = sb("STB", [128, NSTB, 4, 128], BF16)
    ROTFB = sb("ROTFB", [128, 2, 512], F32)
    ROTTB = sb("ROTTB", [128, 2, 4, 64], F32)
    NF, NB = 6, 6
    TMPF = sb("TMPF", [128, NF, 512], F32)
    TMPB = sb("TMPB", [128, NB, 512], BF16)
    RS = sb("RS", [128, 512], F32)
    NM = sb("NM", [128, 512], F32)
    GBE = sb("GBE", [128, T], F32)
    GATE = sb("GATE", [128, NSUB, 8], F32)
    SM = sb("SM", [128, 16, 32], F32)
    NPS = 6
    PS = [nc.alloc_psum_tensor("ps%d" % i, [128, 512], F32) for i in range(NPS)]
    PSM = nc.alloc_psum_tensor("psm", [128, 512], F32)
    PSQ = nc.alloc_psum_tensor("psq", [128, 512], F32)

    B_XT = [[Buf("xt%d_%d" % (g, k)) for k in range(8)] for g in range(NG)]
    B_HT = [Buf("ht%d" % g) for g in range(NG)]
    B_W = [Buf("w%d" % u) for u in range(8)]
    B_CF, B_CB, B_PAR, B_PARA = Buf("cf"), Buf("cb"), Buf("par"), Buf("para")
    B_ST = [Buf("st%d" % l) for l in range(L)]
    B_HIST = [[Buf("hist%d_%d" % (l, c)) for c in range(4)] for l in range(L)]
    B_U = [Buf("u%d" % i) for i in range(4)]
    B_DR = [Buf("dr%d" % i) for i in range(NDR)]
    B_ACC = [Buf("acc%d" % c) for c in range(4)]
    B_MG = [Buf("mg0"), Buf("mg1")]
    B_QR = [Buf("qr0"), Buf("qr1")]
    B_QT = [Buf("qt0"), Buf("qt1")]
    B_KZ = [Buf("kz0"), Buf("kz1")]
    B_KTZ = [Buf("ktz%d" % s) for s in range(4)]
    B_VT = [Buf("vt%d" % s) for s in range(4)]
    B_STB = [Buf("stb%d" % i) for i in range(NSTB)]
    B_ROTF, B_ROTT = Buf("rotf"), Buf("rott")
    B_TF = [Buf("tf%d" % i) for i in range(NF)]
    B_TB = [Buf("tb%d" % i) for i in range(NB)]
    B_RS, B_NM = Buf("rs"), Buf("nm")
    B_GBE = Buf("gbe")
    B_GATE = [Buf("gate%d" % g) for g in range(NG)]
    B_SM = Buf("sm")
    B_PS = [Buf("ps%d" % i, True) for i in range(NPS)]
    B_PSM, B_PSQ = Buf("psm", True), Buf("psq", True)
    B_OUT = Buf("out")

    ctr = {"ps": 0, "tf": 0, "tb": 0, "stb": 0, "u": 0, "dr": 0}

    def bank():
        i = ctr["ps"] % NPS
        ctr["ps"] += 1
        return PS[i], B_PS[i]

    def tf():
        i = ctr["tf"] % NF
        ctr["tf"] += 1
        return TMPF[:, i, :], B_TF[i]

    def tb():
        i = ctr["tb"] % NB
        ctr["tb"] += 1
        return TMPB[:, i, :], B_TB[i]

    def mm(out, lhsT, rhs, start, stop, reads, writes, inc):
        S.op("pe", lambda e: e.matmul(out, lhsT, rhs, start=start, stop=stop), reads, writes, inc)

    def act(out, in_, func, reads, writes, **kw):
        S.op("act", lambda e: e.activation(out=out, in_=in_, func=func, **kw), reads, writes)

    def tt(eng, out, in0, in1, op, reads, writes):
        S.op(eng, lambda e: e.tensor_tensor(out=out, in0=in0, in1=in1, op=op), reads, writes)

    def stt(eng, out, in0, scalar, in1, op0, op1, reads, writes):
        S.op(eng, lambda e: e.scalar_tensor_tensor(out=out, in0=in0, scalar=scalar, in1=in1, op0=op0, op1=op1), reads, writes)

    def ts(eng, out, in0, s1, s2, op0, op1, reads, writes):
        if s2 is None:
            S.op(eng, lambda e: e.tensor_scalar(out=out, in0=in0, scalar1=s1, scalar2=None, op0=op0), reads, writes)
        else:
            S.op(eng, lambda e: e.tensor_scalar(out=out, in0=in0, scalar1=s1, scalar2=s2, op0=op0, op1=op1), reads, writes)

    def cp(eng, out, in_, reads, writes):
        S.op(eng, lambda e: e.tensor_copy(out=out, in_=in_), reads, writes)

    def pcol(name, idx):
        o = PO[name] + idx
        return PAR[:, o:o + 1]

    EPS = CF[:, C_EPS:C_EPS + 1]
    IDENT = CF[:, C_ID:C_ID + 128]
    ONESF = CF[:, C_ONE:C_ONE + 128]
    MASK = CF[:, C_MASK:C_MASK + 512]
    ONES128, ONES512, ONES1024 = CB[:, 0:128], CB[:, 128:256], CB[:, 256:384]

    def Wk(u, n):
        return W[:, u, :].rearrange("p (a b) -> p a b", b=n)

    S.dma("sp", CF[:], cf_d, writes=[B_CF])
    S.dma("sp", PAR[:], par_d, writes=[B_PAR])
    S.dma("pool", CB[:], cb_d, writes=[B_CB])
    DBG = os.environ.get("KDBG", "")
    if "m" not in DBG:
      S.op("pool", lambda e: e.memset(ST[:], 0.0), writes=B_ST)
    if "m" not in DBG:
      S.op("pool", lambda e: e.memset(HIST[:], 0.0), writes=[b for bl in B_HIST for b in bl])
    if "m" not in DBG:
      S.op("pool", lambda e: e.memset(KZ[:], 0.0), writes=B_KZ)
    if "m" not in DBG:
      S.op("pool", lambda e: e.memset(KTZ[:], 0.0), writes=B_KTZ)
    if "m" not in DBG:
      S.op("pool", lambda e: e.memset(STB[:], 0.0), writes=B_STB)
    lnp0 = PO["lnp"]
    ts("dve", PARA[:], PAR[:, lnp0:lnp0 + 4 * L * 8], float(ALPHA), None, ALU.mult, None, [B_PAR], [B_PARA])

    def lnp(which, l, kc, scaled):
        i = (which * L + l) * 8 + kc
        return PARA[:, i:i + 1] if scaled else PAR[:, lnp0 + i:lnp0 + i + 1]

    def finish_stats():
        S.op("act", lambda e: e.activation(out=RS[:], in_=PSM[:], func=AF.Square), [B_PSM], [B_RS])
        tt("dve", RS[:], PSQ[:], RS[:], ALU.subtract, [B_PSQ, B_RS], [B_RS])
        S.op("act", lambda e: e.activation(out=RS[:], in_=RS[:], func=AF.Sqrt, bias=EPS, scale=1.0), [B_RS, B_CF], [B_RS])
        S.op("dve", lambda e: e.reciprocal(out=RS[:], in_=RS[:]), [B_RS], [B_RS])
        stt("dve", NM[:], PSM[:], -1.0, RS[:], ALU.mult, ALU.mult, [B_PSM, B_RS], [B_NM])

    def layernorm(g, which, l, write_ht, scaled):
        cs = slice(g * 512, (g + 1) * 512)
        for kc in range(8):
            xb, bxb = tb()
            act(xb, XT[:, kc, cs], AF.Copy, [B_XT[g][kc]], [bxb])
            mm(PSM[:], ONES1024, xb, kc == 0, kc == 7, [bxb, B_CB], [B_PSM], True)
            xq, bxq = tb()
            act(xq, XT[:, kc, cs], AF.Square, [B_XT[g][kc]], [bxq])
            mm(PSQ[:], ONES1024, xq, kc == 0, kc == 7, [bxq, B_CB], [B_PSQ], True)
        finish_stats()
        for kc0 in range(0, 8, 2):
          tl = [tf(), tf()]
          for i_ in range(2):
            tt("dve", tl[i_][0], XT[:, kc0 + i_, cs], RS[:], ALU.mult, [B_XT[g][kc0 + i_], B_RS], [tl[i_][1]])
          for i_ in range(2):
            tt("dve", tl[i_][0], tl[i_][0], NM[:], ALU.add, [tl[i_][1], B_NM], [tl[i_][1]])
          for i_ in range(2):
            kc = kc0 + i_
            t, bt = tl[i_]
            if write_ht:
                act(HT[:, kc, cs], t, AF.Identity, [bt, B_PAR], [B_HT[g]], scale=lnp(which, l, kc, False), bias=lnp(which + 1, l, kc, False))
            act(XT[:, kc, cs], t, AF.Identity, [bt, B_PAR, B_PARA], [B_XT[g][kc]],
                scale=lnp(which, l, kc, scaled), bias=lnp(which + 1, l, kc, scaled))

    def load_tile(ti):
        for s in range(NSUB):
            g, s4 = s // 4, s % 4
            io = ACC[:, 2 * (s % 2):2 * (s % 2) + 2, :].rearrange("p a b -> p (a b)")
            bio = [B_ACC[2 * (s % 2)], B_ACC[2 * (s % 2) + 1]]
            r0 = ti * T + s * 128
            S.dma("sp", io, x_d[r0:r0 + 128, :], writes=bio)
            for kcb in range(2):
                pb, bpb = bank()
                for k4 in range(4):
                    kc = kcb * 4 + k4
                    S.op("pe", lambda e, pb=pb, k4=k4, kc=kc, io=io: e.transpose(pb[:, k4 * 128:(k4 + 1) * 128], io[:, kc * 128:(kc + 1) * 128], IDENT),
                         bio + [B_CF], [bpb], inc=(k4 == 3))
                pv = pb[:].rearrange("p (a b) -> p a b", b=128)
                ts_ = slice(s * 128, (s + 1) * 128)
                if "a" not in DBG:
                    act(HT[:, kcb * 4:(kcb + 1) * 4, ts_], pv, AF.Copy, [bpb], [B_HT[g]])
                if "v" not in DBG:
                    ts("dve", XT[:, kcb * 4:(kcb + 1) * 4, ts_], pv, float(ALPHA), None, ALU.mult, None, [bpb], B_XT[g][kcb * 4:(kcb + 1) * 4])

    def store_tile(ti):
        for s in range(NSUB):
            g = s // 4
            io = ACC[:, 2 * (s % 2):2 * (s % 2) + 2, :]
            bio = [B_ACC[2 * (s % 2)], B_ACC[2 * (s % 2) + 1]]
            for kcb in range(2):
                pb, bpb = bank()
                for k4 in range(4):
                    kc = kcb * 4 + k4
                    S.op("pe", lambda e, pb=pb, k4=k4, kc=kc, s=s: e.transpose(pb[:, k4 * 128:(k4 + 1) * 128], XT[:, kc, s * 128:(s + 1) * 128], IDENT),
                         [B_XT[g][kc], B_CF], [bpb], inc=(k4 == 3))
                if kcb == 0:
                    act(io[:, 0, :], pb[:], AF.Copy, [bpb], [bio[0]])
                else:
                    cp("dve", io[:, 1, :], pb[:], [bpb], [bio[1]])
            r0 = ti * T + s * 128
            S.dma("sp", out_d[r0:r0 + 128, :], io.rearrange("p a b -> p (a b)"), reads=bio, writes=[B_OUT])

    MU = {}
    SL = {"E": 0, "F": 1}

    def load_mixer_weights(l):
        E, F = SL["E"], SL["F"]
        MU.update({"q": 6, "k": 7, "ga": 3 * E, "gb": 3 * E + 1, "g": 3 * E + 2, "v": 3 * F, "o0": 3 * F + 1, "o1": 3 * F + 2})
        for nm_, j in (("q", 2), ("k", 3), ("ga", 0), ("gb", 1), ("g", 5), ("v", 4)):
            S.dma("pool", W[:, MU[nm_], :], win_d[l, j], writes=[B_W[MU[nm_]]])
        for j in range(2):
            S.dma("pool", W[:, MU["o%d" % j], :], wout_d[l, j], writes=[B_W[MU["o%d" % j]]])

    def load_slab(wd, idx, slot):
        for k in range(3):
            S.dma("pool", W[:, 3 * slot + k, :], wd[k][idx], writes=[B_W[3 * slot + k]])

    def mixer_group(ti, l, g):
        cs = slice(g * 512, (g + 1) * 512)
        tok0 = ti * T + g * 512
        bht = B_HT[g]
        S.dma("sp", ROTFB[:], rotf_d[:, :, tok0:tok0 + 512].rearrange("c p t -> p c t"), writes=[B_ROTF])
        for c_ in range(2):
            S.dma("sp", ROTTB[:, c_, :, :], rott_d[c_, tok0:tok0 + 512, :].rearrange("(s p) d -> p s d", p=128), writes=[B_ROTT])
        W0, W1, W2, W3, W4, W5 = (Wk(MU[n_], 512) for n_ in ("ga", "gb", "q", "k", "v", "g"))
        W6, W7 = Wk(MU["o0"], 1024), Wk(MU["o1"], 1024)
        BW0, BW1, BW2, BW3, BW4, BW5, BW6, BW7 = (B_W[MU[n_]] for n_ in ("ga", "gb", "q", "k", "v", "g", "o0", "o1"))

        def proj_fm(wv, bw, c0):
            pb, bpb = bank()
            for kc in range(8):
                mm(pb[:], wv[:, kc, c0:c0 + 128], HT[:, kc, cs], kc == 0, kc == 7, [bw, bht], [bpb], kc == 7)
            return pb, bpb

        if cfg.stage < 1:
            return
        for hp in range(2):
            pq, bq = proj_fm(W2, BW2, hp * 128)
            pqp, bqp = proj_fm(W2, BW2, 256 + hp * 128)
            f1, b1 = tf()
            f2, b2 = tf()
            tt("dve", f1, pq[:], ROTFB[:, 0, :], ALU.mult, [bq, B_ROTF], [b1])
            tt("dve", f2, pqp[:], ROTFB[:, 1, :], ALU.mult, [bqp, B_ROTF], [b2])
            tt("dve", f1, f1, f2, ALU.add, [b1, b2], [b1])
            act(QR[:, hp, :], f1, AF.Copy, [b1], [B_QR[hp]])
            qd = CF[:, C_QD + hp * 64:C_QD + (hp + 1) * 64].unsqueeze(1).broadcast_to([128, 8, 64])
            tt("dve", QT[:, hp, :].rearrange("p (c i) -> p c i", i=64), f1.rearrange("p (c i) -> p c i", i=64), qd, ALU.mult,
               [b1, B_CF], [B_QT[hp]])
        for hp in range(2):
            pk, bk = proj_fm(W3, BW3, hp * 128)
            pkp, bkp = proj_fm(W3, BW3, 256 + hp * 128)
            f1, b1 = tf()
            f2, b2 = tf()
            tt("dve", f1, pk[:], ROTFB[:, 0, :], ALU.mult, [bk, B_ROTF], [b1])
            tt("dve", f2, pkp[:], ROTFB[:, 1, :], ALU.mult, [bkp, B_ROTF], [b2])
            for hh in range(2):
                rows = slice(hh * 64, (hh + 1) * 64)
                tt("dve", KZ[rows, hp, hh, :], f1[rows], f2[rows], ALU.add, [b1, b2], [B_KZ[hp]])
        if cfg.stage < 2:
            return
        for s in range(4):
            tsl = slice(g * 512 + s * 128, g * 512 + (s + 1) * 128)
            pkt, bkt = bank()
            for kc in range(8):
                mm(pkt[:], HT[:, kc, tsl], W3[:, kc, :], kc == 0, kc == 7, [BW3, bht], [bkt], kc == 7)
            pv, bv = bank()
            for kc in range(8):
                mm(pv[:], HT[:, kc, tsl], W4[:, kc, :], kc == 0, kc == 7, [BW4, bht], [bv], kc == 7)
            act(VT[:, s, :], pv[:], AF.Copy, [bv], [B_VT[s]])
            f1, b1 = tf()
            f2, b2 = tf()
            cosb = ROTTB[:, 0, s, :].unsqueeze(1).broadcast_to([128, 4, 64])
            sinb = ROTTB[:, 1, s, :].unsqueeze(1).broadcast_to([128, 4, 64])
            tt("dve", f1[:, 0:256].rearrange("p (h d) -> p h d", d=64), pkt[:, 0:256].rearrange("p (h d) -> p h d", d=64), cosb, ALU.mult, [bkt, B_ROTT], [b1])
            tt("dve", f2[:, 0:256].rearrange("p (h d) -> p h d", d=64), pkt[:, 256:512].rearrange("p (h d) -> p h d", d=64), sinb, ALU.mult, [bkt, B_ROTT], [b2])
            tt("dve", f1[:, 0:256], f1[:, 0:256], f2[:, 0:256], ALU.add, [b1, b2], [b1])
            for c in range(2):
                rows = slice(c * 64, (c + 1) * 64)
                tt("dve", KTZ[rows, c, s, :], f1[rows, 0:256], CF[rows, C_KD:C_KD + 256], ALU.mult, [b1, B_CF], [B_KTZ[s]])
        if cfg.stage < 2:
            return
        for c in range(4):
            pa, ba = proj_fm(W0, BW0, c * 128)
            pbk, bb = proj_fm(W1, BW1, c * 128)
            sg, bsg = tf()
            act(sg, pbk[:], AF.Sigmoid, [bb], [bsg])
            Uc, bu = U[:, c, :], B_U[c]
            act(Uc[:, 0:30], HIST[:, l, c, :], AF.Copy, [B_HIST[l][c]], [bu])
            tt("dve", Uc[:, 30:542], pa[:], sg, ALU.mult, [ba, bsg], [bu])
            act(HIST[:, l, c, :], Uc[:, 512:542], AF.Copy, [bu], [B_HIST[l][c]])
        for h in range(4):
            pg, bg = proj_fm(W5, BW5, h * 128)
            act(MG[:, 1, h, :], pg[:], AF.Silu, [bg], [B_MG[1]])
        if cfg.stage < 5:
            return
        cvb = {}
        CONV_SCHED = [[(0, 0, 16), (0, 16, CONV_K), (1, 0, 16)], [(1, 16, CONV_K), (2, 0, 16), (2, 16, CONV_K)],
                      [(3, 0, 16), (3, 16, CONV_K), None], [None, None, None]]

        dslot = {}

        def conv_build(c, j0, j1):
            for j in range(j0, j1):
                slot = ctr["dr"] % NDR
                ctr["dr"] += 1
                dslot[(c, j)] = slot
                wj = pcol("cw", (l * 4 + c) * CONV_K + j)
                if j % 2 == 0:
                    act(DR[:, slot, :], IDENT, AF.Identity, [B_CF, B_PAR], [B_DR[slot]], scale=wj)
                else:
                    ts("dve", DR[:, slot, :], IDENT, wj, None, ALU.mult, None, [B_CF, B_PAR], [B_DR[slot]])

        def conv_taps(c, j0, j1):
            if j0 == 0:
                cvb[c] = bank()
            pcv, bcv = cvb[c]
            for j in range(j0, j1):
                slot = dslot[(c, j)]
                mm(pcv[:], DR[:, slot, :], U[:, c, j:j + 512], j == 0, j == CONV_K - 1, [B_DR[slot], B_U[c]], [bcv], True)
            if j1 == CONV_K:
                acc, bacc = ACC[:, c, :], B_ACC[c]
                act(acc, pcv[:], AF.Identity, [bcv, B_PAR], [bacc], bias=pcol("cb", l * 4 + c), scale=1.0)
                xb, bxb = tb()
                act(xb, acc, AF.Copy, [bacc], [bxb])
                mm(PSM[:], ONES512, xb, c == 0, c == 3, [bxb, B_CB], [B_PSM], True)
                xq, bxq = tb()
                act(xq, acc, AF.Square, [bacc], [bxq])
                mm(PSQ[:], ONES512, xq, c == 0, c == 3, [bxq, B_CB], [B_PSQ], True)

        for s in range(4):
            ssl = slice(s * 128, (s + 1) * 128)
            slots = []
            pcs = CONV_SCHED[s]
            if pcs[0]:
                conv_build(*pcs[0])
            for c in range(2):
                pkv, bkv = bank()
                for hp in range(2):
                    mm(pkv[:, hp * 256:(hp + 1) * 256], KTZ[:, c, s, hp * 128:(hp + 1) * 128], VT[:, s, hp * 256:(hp + 1) * 256],
                       True, True, [B_KTZ[s], B_VT[s]], [bkv], hp == 1)
                slot = ctr["stb"] % NSTB
                ctr["stb"] += 1
                slots.append(slot)
                for h in range(4):
                    hp, hh = h // 2, h % 2
                    rows = slice(hh * 64, (hh + 1) * 64)
                    act(STB[rows, slot, h, :], ST[rows, l, hp, :], AF.Copy, [B_ST[l]], [B_STB[slot]])
                for h in range(4):
                    hp, hh = h // 2, h % 2
                    rows = slice(hh * 64, (hh + 1) * 64)
                    stt("dve", ST[rows, l, hp, :], ST[rows, l, hp, :], CF[rows, C_CDS + hp:C_CDS + hp + 1],
                        pkv[rows, hp * 256 + hh * 128:hp * 256 + (hh + 1) * 128], ALU.mult, ALU.add, [B_ST[l], B_CF, bkv], [B_ST[l]])
            if pcs[0]:
                conv_taps(*pcs[0])
            if pcs[1]:
                conv_build(*pcs[1])
            psc, bsc = bank()
            for h in range(4):
                hp, hh = h // 2, h % 2
                mm(psc[:, h * 128:(h + 1) * 128], KZ[:, hp, hh, ssl], QR[:, hp, ssl], True, True, [B_KZ[hp], B_QR[hp]], [bsc], h == 3)
            smt, bsm = tb()
            tt("dve", smt, psc[:], MASK, ALU.mult, [bsc, B_CF], [bsm])
            if pcs[1]:
                conv_taps(*pcs[1])
            if pcs[2]:
                conv_build(*pcs[2])
            po, bo = bank()
            for h in range(4):
                hp = h // 2
                mm(po[:, h * 128:(h + 1) * 128], VT[:, s, h * 128:(h + 1) * 128], smt[:, h * 128:(h + 1) * 128], True, False,
                   [B_VT[s], bsm], [bo], False)
                for c in range(2):
                    mm(po[:, h * 128 + c * 64:h * 128 + (c + 1) * 64], STB[:, slots[c], h, :], QT[:, hp, s * 128 + c * 64:s * 128 + (c + 1) * 64],
                       False, True, [B_STB[slots[c]], B_QT[hp]], [bo], (h == 3 and c == 1))
            if pcs[2]:
                conv_taps(*pcs[2])
            ob, bob = tb()
            act(ob, po[:], AF.Copy, [bo], [bob])
            pm, bpm = bank()
            mm(pm[:], ONES128, ob, True, True, [bob, B_CB], [bpm], True)
            oq, boq = tb()
            act(oq, po[:], AF.Square, [bo], [boq])
            pq, bpq = bank()
            mm(pq[:], ONES128, oq, True, True, [boq, B_CB], [bpq], True)
            rs, brs = tf()
            nmv, bnm = tf()
            act(rs, pm[:], AF.Square, [bpm], [brs])
            tt("dve", rs, pq[:], rs, ALU.subtract, [bpq, brs], [brs])
            act(rs, rs, AF.Sqrt, [brs, B_CF], [brs], bias=EPS, scale=1.0)
            S.op("dve", lambda e, rs=rs: e.reciprocal(out=rs, in_=rs), [brs], [brs])
            stt("dve", nmv, pm[:], -1.0, rs, ALU.mult, ALU.mult, [bpm, brs], [bnm])
            y, by = tf()
            tt("dve", y, po[:], rs, ALU.mult, [bo, brs], [by])
            tt("dve", y, y, nmv, ALU.add, [by, bnm], [by])
            gng = PAR[:, PO["gng"] + l * 4:PO["gng"] + l * 4 + 4].unsqueeze(2).broadcast_to([128, 4, 128])
            y3 = y.rearrange("p (h i) -> p h i", i=128)
            tt("dve", y3, y3, gng, ALU.mult, [by, B_PAR], [by])
            mgv = MG[:, 1, :, s * 128:(s + 1) * 128]
            tt("dve", mgv, y3, mgv, ALU.mult, [by, B_MG[1]], [B_MG[1]])
            if s == 2:
                finish_stats()
                for c in range(4):
                    acc, bacc = ACC[:, c, :], B_ACC[c]
                    tt("dve", acc, acc, RS[:], ALU.mult, [bacc, B_RS], [bacc])
                    tt("dve", acc, acc, NM[:], ALU.add, [bacc, B_NM], [bacc])
                    act(MG[:, 0, c, :], acc, AF.Silu, [bacc, B_PAR], [B_MG[0]], scale=pcol("clg", l * 4 + c), bias=pcol("clb", l * 4 + c))
        if cfg.stage < 6:
            return
        for dc in range(8):
            py, bpy = bank()
            for mc in range(8):
                wv, bw = (W6, BW6) if mc < 4 else (W7, BW7)
                mm(py[:], wv[:, mc % 4, dc * 128:(dc + 1) * 128], MG[:, mc // 4, mc % 4, :], mc == 0, mc == 7, [bw, B_MG[mc // 4]], [bpy], mc == 7)
            tt("dve", XT[:, dc, cs], py[:], XT[:, dc, cs], ALU.add, [bpy, B_XT[g][dc]], [B_XT[g][dc]])
        layernorm(g, 0, l, True, True)

    def routing_group(li, g):
        plg, blg = bank()
        for s in range(4):
            tsl = slice(g * 512 + s * 128, g * 512 + (s + 1) * 128)
            for kc in range(8):
                ro = PO["rout"] + (li * 8 + kc) * 8
                mm(plg[:, s * 8:(s + 1) * 8], XT[:, kc, tsl], PAR[:, ro:ro + 8], kc == 0, kc == 7, [B_XT[g][kc], B_PAR], [blg], (s == 3 and kc == 7))
        lg = SM[:, 0, :].rearrange("p (s e) -> p s e", e=8)
        eq1 = SM[:, 1, :].rearrange("p (s e) -> p s e", e=8)
        l2 = SM[:, 2, :].rearrange("p (s e) -> p s e", e=8)
        eq2 = SM[:, 3, :].rearrange("p (s e) -> p s e", e=8)
        m1, m2, dd, ee, p1, p2 = (SM[:, 4 + i, 0:4] for i in range(6))
        bs = [B_SM]
        cp("dve", SM[:, 0, :], plg[:, 0:32], [blg], bs)
        S.op("dve", lambda e: e.tensor_reduce(out=m1, in_=lg, axis=AX.X, op=ALU.max), bs, bs)
        tt("dve", eq1, lg, m1.unsqueeze(2).broadcast_to([128, 4, 8]), ALU.is_equal, bs, bs)
        stt("dve", l2, eq1, -1e30, lg, ALU.mult, ALU.add, bs, bs)
        S.op("dve", lambda e: e.tensor_reduce(out=m2, in_=l2, axis=AX.X, op=ALU.max), bs, bs)
        tt("dve", eq2, l2, m2.unsqueeze(2).broadcast_to([128, 4, 8]), ALU.is_equal, bs, bs)
        tt("dve", dd, m2, m1, ALU.subtract, bs, bs)
        act(ee, dd, AF.Exp, bs, bs, scale=float(1.0 / ALPHA))
        ts("dve", p1, ee, 1.0, None, ALU.add, None, bs, bs)
        S.op("dve", lambda e: e.reciprocal(out=p1, in_=p1), bs, bs)
        tt("dve", p2, ee, p1, ALU.mult, bs, bs)
        tt("dve", eq1, eq1, p1.unsqueeze(2).broadcast_to([128, 4, 8]), ALU.mult, bs, bs)
        tt("dve", eq2, eq2, p2.unsqueeze(2).broadcast_to([128, 4, 8]), ALU.mult, bs, bs)
        tt("dve", GATE[:, g * 4:(g + 1) * 4, :], eq1, eq2, ALU.add, bs, [B_GATE[g]])

    def build_gbe(ex):
        for g in range(NG):
            pb, bpb = bank()
            r, br = tf()
            for s in range(4):
                ts("dve", r[:, s * 128:(s + 1) * 128], IDENT, GATE[:, g * 4 + s, ex:ex + 1], None, ALU.mult, None, [B_CF, B_GATE[g]], [br])
            for s in range(4):
                mm(pb[:, s * 128:(s + 1) * 128], ONESF, r[:, s * 128:(s + 1) * 128], True, True, [br, B_CF], [bpb], s == 3)
            act(GBE[:, g * 512:(g + 1) * 512], pb[:], AF.Copy, [bpb], [B_GBE])

    def ffn_tile(l):
        moe = cfg.layer_is_moe[l]
        li = sum(1 for m in cfg.layer_is_moe[:l] if m == moe)
        wd = mw_d if moe else dw_d
        nex = NE if moe else 1
        slabs = [(ex, sl) for ex in range(nex) for sl in range(NSLAB)]
        base = li * nex * NSLAB
        E_, F_ = SL["E"], SL["F"]
        slot_of = lambda i: (E_ if i % 2 == 0 else F_)
        load_slab(wd, base + 0, slot_of(0))
        if len(slabs) > 1:
            load_slab(wd, base + 1, slot_of(1))
        items = [(i, g) for i in range(len(slabs)) for g in range(NG)]

        def phase_a(i, g, gi):
            ex, sl = slabs[i]
            slot = slot_of(i)
            cs = slice(g * 512, (g + 1) * 512)
            if moe and sl == 0 and g == 0:
                build_gbe(ex)
            W1s, W3s = Wk(3 * slot, 512), Wk(3 * slot + 1, 512)
            for fc in range(4):
                pa, ba = bank()
                for kc in range(8):
                    mm(pa[:], W1s[:, kc, fc * 128:(fc + 1) * 128], HT[:, kc, cs], kc == 0, kc == 7, [B_W[3 * slot], B_HT[g]], [ba], kc == 7)
                pc, bc = bank()
                for kc in range(8):
                    mm(pc[:], W3s[:, kc, fc * 128:(fc + 1) * 128], HT[:, kc, cs], kc == 0, kc == 7, [B_W[3 * slot + 1], B_HT[g]], [bc], kc == 7)
                sa, bsa = tf()
                act(sa, pa[:], AF.Silu, [ba], [bsa])
                if moe:
                    tt("dve", sa, pc[:], sa, ALU.mult, [bc, bsa], [bsa])
                    tt("dve", MG[:, gi % 2, fc, :], sa, GBE[:, cs], ALU.mult, [bsa, B_GBE], [B_MG[gi % 2]])
                else:
                    tt("dve", MG[:, gi % 2, fc, :], pc[:], sa, ALU.mult, [bc, bsa], [B_MG[gi % 2]])

        def phase_b(i, g, gi):
            slot = slot_of(i)
            cs = slice(g * 512, (g + 1) * 512)
            W2s = Wk(3 * slot + 2, 1024)
            for dc in range(8):
                py, bpy = bank()
                for fc in range(4):
                    mm(py[:], W2s[:, fc, dc * 128:(dc + 1) * 128], MG[:, gi % 2, fc, :], fc == 0, fc == 3, [B_W[3 * slot + 2], B_MG[gi % 2]], [bpy], fc == 3)
                tt("dve", XT[:, dc, cs], py[:], XT[:, dc, cs], ALU.add, [bpy, B_XT[g][dc]], [B_XT[g][dc]])
            if g == NG - 1 and i + 2 < len(slabs):
                load_slab(wd, base + i + 2, slot)

        for gi, (i, g) in enumerate(items):
            phase_a(i, g, gi)
            if gi > 0:
                pi, pg = items[gi - 1]
                phase_b(pi, pg, gi - 1)
        pi, pg = items[-1]
        phase_b(pi, pg, len(items) - 1)
        SL["E"], SL["F"] = slot_of(len(slabs) - 2), slot_of(len(slabs) - 1)

    for ti in range(NT):
        if "l" not in DBG:
            load_tile(ti)
        for l in range(L):
            if cfg.stage < -1:
                continue
            load_mixer_weights(l)
            if cfg.stage < 0:
                continue
            for g in range(NG):
                mixer_group(ti, l, g)
                if cfg.layer_is_moe[l]:
                    li = sum(1 for m in cfg.layer_is_moe[:l] if m)
                    routing_group(li, g)
            if cfg.stage < 7:
                continue
            ffn_tile(l)
            last = (l == L - 1)
            for g in range(NG):
                layernorm(g, 2, l, not last, not last)
        if "s" not in DBG:
            store_tile(ti)
    S.wait_all("sp", [B_OUT])
    S.emit()
    return nc, S


def host_constants(S_):
    half = RET_DK // 2
    inv_freq = (np.float32(10000.0) ** (-np.arange(half, dtype=np.float32) / np.float32(half))).astype(np.float32)
    pos = np.arange(S_, dtype=np.float32)
    ang = (pos[:, None] * inv_freq[None, :]).astype(np.float32)
    cos, sin = np.cos(ang).astype(np.float32), np.sin(ang).astype(np.float32)
    cos64 = np.concatenate([cos, cos], axis=1)
    sin64 = np.concatenate([-sin, sin], axis=1)
    rott = np.ascontiguousarray(np.stack([cos64, sin64], axis=0))
    rotf = np.ascontiguousarray(np.stack([np.tile(cos64.T, (2, 1)), np.tile(sin64.T, (2, 1))], axis=0))
    h = np.arange(RET_HEADS, dtype=np.float32)
    log_gamma = np.log1p(-(np.float32(2.0) ** (-5.0 - h))).astype(np.float32)
    idx = np.arange(CHUNK, dtype=np.float32)
    cf = np.zeros((128, NCF), np.float32)
    p = np.arange(128)
    for hh in range(RET_HEADS):
        d = np.abs(p[:, None] - p[None, :]).astype(np.float32)
        m = np.exp(log_gamma[hh] * d).astype(np.float32) * np.float32(RET_DK ** -0.5)
        m = m * ((p[:, None] // 64) == (p[None, :] // 64))
        cf[:, C_MASK + hh * 128:C_MASK + (hh + 1) * 128] = m
    for hp in range(2):
        for hh in range(2):
            cf[hh * 64:(hh + 1) * 64, C_QD + hp * 64:C_QD + (hp + 1) * 64] = np.exp(log_gamma[2 * hp + hh] * (idx + 1.0))[None, :]
            cf[hh * 64:(hh + 1) * 64, C_CDS + hp] = np.exp(log_gamma[2 * hp + hh] * np.float32(CHUNK))
    for hh in range(RET_HEADS):
        kd = np.exp(log_gamma[hh] * (CHUNK - 1.0 - (p % 64).astype(np.float32))).astype(np.float32) * np.float32(RET_DK ** -0.5)
        cf[:, C_KD + hh * 64:C_KD + (hh + 1) * 64] = kd[:, None]
    cf[:, C_ID:C_ID + 128] = np.eye(128, dtype=np.float32)
    cf[:, C_ONE:C_ONE + 128] = 1.0
    cf[:, C_EPS] = LN_EPS
    cb = np.zeros((128, 384), np.float32)
    cb[:, 0:128] = 1.0 / 128
    cb[:, 128:256] = 1.0 / 512
    cb[:, 256:384] = 1.0 / 1024
    return rotf, rott, cf, cb


def _slab_w13(w):
    lead = w.shape[:-2]
    w = w.reshape(lead + (8, 128, NSLAB, 512))
    n = len(lead)
    w = np.transpose(w, tuple(range(n)) + (n + 2, n + 1, n + 0, n + 3))
    return np.ascontiguousarray(w).reshape((-1, 128, 4096))


def _slab_w2(w):
    lead = w.shape[:-2]
    w = w.reshape(lead + (NSLAB, 4, 128, 1024))
    n = len(lead)
    w = np.transpose(w, tuple(range(n)) + (n + 0, n + 2, n + 1, n + 3))
    return np.ascontiguousarray(w).reshape((-1, 128, 4096))


def prepare_shared(cfg, w_in, w_out, conv_w, conv_b, conv_ln_g, conv_ln_b, ret_gn_g, ln1_g, ln1_b, ln2_g, ln2_b,
                   dense_w1, dense_w3, dense_w2, moe_router, moe_w1, moe_w3, moe_w2):
    L = cfg.L
    f = lambda a: np.asarray(a, dtype=np.float32)
    w_in, w_out = f(w_in)[:L], f(w_out)[:L]
    perm = np.concatenate([(np.arange(64) + 32) % 64 + 64 * hh for hh in range(RET_HEADS)])
    ga, gb = w_in[:, :, 0:512], w_in[:, :, 512:1024]
    q, k = w_in[:, :, 1024:1280], w_in[:, :, 1280:1536]
    v, g = w_in[:, :, 1536:2048], w_in[:, :, 2048:2560]
    ext = np.concatenate([ga, gb, q, q[:, :, perm], k, k[:, :, perm], v, g], axis=2)
    win = np.ascontiguousarray(ext.reshape(L, 8, 128, 6, 512).transpose(0, 3, 2, 1, 4)).reshape(L, 6, 128, 4096)
    wout = np.ascontiguousarray(w_out.reshape(L, 2, 4, 128, 1024).transpose(0, 1, 3, 2, 4)).reshape(L, 2, 128, 4096)
    nd, nm = max(cfg.n_dense, 1), max(cfg.n_moe, 1)
    dw = [_slab_w13(f(dense_w1)[:nd]), _slab_w13(f(dense_w3)[:nd]), _slab_w2(f(dense_w2)[:nd])]
    mw = [_slab_w13(f(moe_w1)[:nm, :cfg.NE]), _slab_w13(f(moe_w3)[:nm, :cfg.NE]), _slab_w2(f(moe_w2)[:nm, :cfg.NE])]
    PO = par_layout(L, cfg.n_moe)
    par = np.zeros((128, PO["_n"]), np.float32)
    cw = f(conv_w)[:L].reshape(L, CONV_K, 4, 128).transpose(3, 0, 2, 1)
    par[:, PO["cw"]:PO["cw"] + L * 4 * CONV_K] = cw.reshape(128, -1)
    for name, a in (("cb", conv_b), ("clg", conv_ln_g), ("clb", conv_ln_b), ("gng", ret_gn_g)):
        par[:, PO[name]:PO[name] + L * 4] = f(a)[:L].reshape(L, 4, 128).transpose(2, 0, 1).reshape(128, -1)
    lnp = np.stack([f(a)[:L].reshape(L, 8, 128).transpose(2, 0, 1) for a in (ln1_g, ln1_b, ln2_g, ln2_b)], axis=1)
    par[:, PO["lnp"]:PO["lnp"] + 4 * L * 8] = lnp.reshape(128, -1)
    if cfg.n_moe > 0:
        ro = f(moe_router)[:cfg.n_moe].reshape(cfg.n_moe, 8, 128, 8).transpose(2, 0, 1, 3)
        par[:, PO["rout"]:PO["rout"] + cfg.n_moe * 64] = ro.reshape(128, -1)
    rotf, rott, cf, cb = host_constants(cfg.S)
    sh = {"win": win, "wout": wout, "par": par, "rotf": rotf, "rott": rott, "constf": cf, "constb": cb}
    for i, kk in enumerate((1, 3, 2)):
        sh["dw%d" % kk] = dw[i]
        sh["mw%d" % kk] = mw[i]
    return sh


def run(cfg, x, **weights):
    nc, S = build_program(cfg)
    sh = prepare_shared(cfg, **weights)
    x = np.asarray(x, dtype=np.float32)
    ncore = x.shape[0]
    in_maps = []
    for c in range(ncore):
        m = dict(sh)
        m["x"] = np.ascontiguousarray(x[c])
        in_maps.append(m)
    if os.environ.get("KTRACE"):
        res = run_bass_kernel_spmd(nc, in_maps, core_ids=list(range(ncore)), trace=True)
        print("EXEC_NS", res.exec_time_ns, flush=True)
    else:
        res = run_bass_kernel_spmd(nc, in_maps, core_ids=list(range(ncore)))
    return np.stack([np.asarray(r["out"]) for r in res.results], axis=0).astype(np.float32)


def kernel(x, w_in, w_out, conv_w, conv_b, conv_ln_g, conv_ln_b, ret_gn_g, ln1_g, ln1_b, ln2_g, ln2_b,
           dense_w1, dense_w3, dense_w2, moe_router, moe_w1, moe_w3, moe_w2):
    cfg = Cfg(S=int(np.shape(x)[1]), T=1024, layers=DEPTH)
    return run(cfg, x, w_in=w_in, w_out=w_out, conv_w=conv_w, conv_b=conv_b, conv_ln_g=conv_ln_g, conv_ln_b=conv_ln_b,
               ret_gn_g=ret_gn_g, ln1_g=ln1_g, ln1_b=ln1_b, ln2_g=ln2_g, ln2_b=ln2_b, dense_w1=dense_w1, dense_w3=dense_w3,
               dense_w2=dense_w2, moe_router=moe_router, moe_w1=moe_w1, moe_w3=moe_w3, moe_w2=moe_w2)
```
